# Optimizing a Trainium2 kernel written in Bass

```python
import math
import jax, jax.numpy as jnp
from jax import lax
import numpy as np

D_MODEL = 1024
BATCH = 8
SEQ = 4096
DEPTH = 1

HEAD_DIM = 64
N_HEADS = D_MODEL // HEAD_DIM
N_HEADS_A = N_HEADS // 2
N_HEADS_B = N_HEADS - N_HEADS_A
WIDTH_A = N_HEADS_A * HEAD_DIM
WIDTH_B = N_HEADS_B * HEAD_DIM
DILATED_PAIRS = ((128, 1), (512, 4), (2048, 16))
MOBA_BLOCK = 256
MOBA_TOPK = 3
MOBA_QUERY_CHUNK = 16
D_FF = 4 * D_MODEL
PLE_DIM = 256
EPS = 1e-6

kernel_name = 'hybrid_dilated_moba_block'


def alibi_slopes(n):
    return jnp.asarray(np.array([2.0 ** (-8.0 * (i + 1) / n) for i in range(n)], dtype=np.float32))


def rmsnorm(x, g):
    xf = x.astype(jnp.float32)
    y = xf * lax.rsqrt(jnp.mean(xf * xf, axis=-1, keepdims=True) + EPS)
    return (y * g.astype(jnp.float32)).astype(x.dtype)


def dilated_branch(q, k, v, slopes, window, dilation):
    b, h, s, hd = q.shape
    nk = window // dilation
    c = nk
    span = dilation * c
    s_pad = -(-s // span) * span
    L = s_pad // dilation
    nc = L // c

    def to_sub(t):
        t = jnp.pad(t, ((0, 0), (0, 0), (0, s_pad - s), (0, 0))).reshape(b, h, L, dilation, hd)
        return jnp.swapaxes(t, 2, 3).reshape(b, h, dilation, nc, c, hd)

    def with_prev(t):
        prev = jnp.pad(t[:, :, :, :-1], ((0, 0), (0, 0), (0, 0), (1, 0), (0, 0), (0, 0)))
        return jnp.concatenate([prev, t], axis=4)

    qs, ks, vs = to_sub(q), to_sub(k), to_sub(v)
    kk, vv = with_prev(ks), with_prev(vs)
    scores = jnp.einsum('bhrnqd,bhrnkd->bhrnqk', qs, kk,
                        preferred_element_type=jnp.float32) * (hd ** -0.5)
    qi = jnp.arange(c)[:, None]
    ki = jnp.arange(2 * c)[None, :]
    m = c + qi - ki
    key_sub = jnp.arange(nc)[:, None, None] * c + ki[None] - c
    valid = (m >= 0) & (m <= nk) & (key_sub >= 0)
    bias = -slopes[None, :, None, None, None, None] * (m * dilation).astype(jnp.float32)
    scores = jnp.where(valid, scores + bias, -jnp.inf)
    lse = jax.nn.logsumexp(scores, axis=-1)
    probs = jnp.exp(scores - lse[..., None])
    out = jnp.einsum('bhrnqk,bhrnkd->bhrnqd', probs, vv, preferred_element_type=jnp.float32)

    def from_sub(t):
        rest = t.shape[5:]
        t = jnp.swapaxes(t.reshape((b, h, dilation, L) + rest), 2, 3)
        return t.reshape((b, h, s_pad) + rest)[:, :, :s]

    return from_sub(out), from_sub(lse)


def dilated_mixture(q, k, v, slopes):
    outs, lses = [], []
    for window, dilation in DILATED_PAIRS:
        o, l = dilated_branch(q, k, v, slopes, window, dilation)
        outs.append(o)
        lses.append(l)
    w = jax.nn.softmax(jnp.stack(lses, 0), axis=0)
    return jnp.sum(w[..., None] * jnp.stack(outs, 0), axis=0)


def moba_attention(q, k, v, slopes):
    b, h, s, hd = q.shape
    bs = MOBA_BLOCK
    s_pad = -(-s // bs) * bs
    nblk = s_pad // bs
    pad = ((0, 0), (0, 0), (0, s_pad - s), (0, 0))
    q, k, v = jnp.pad(q, pad), jnp.pad(k, pad), jnp.pad(v, pad)
    kb = k.reshape(b, h, nblk, bs, hd)
    vb = v.reshape(b, h, nblk, bs, hd)
    kmean = jnp.mean(kb.astype(jnp.float32), axis=3)
    pos = jnp.arange(s_pad)
    own = pos // bs
    gate = jnp.einsum('bhsd,bhnd->bhsn', q.astype(jnp.float32), kmean)
    past = jnp.arange(nblk)[None, :] < own[:, None]
    gate = jnp.where(past, gate, -jnp.inf)
    ksel = min(MOBA_TOPK, nblk)
    _, top_idx = lax.top_k(gate, ksel)
    top_valid = top_idx < own[:, None]
    own_b = jnp.broadcast_to(own[None, None, :, None], (b, h, s_pad, 1)).astype(top_idx.dtype)
    sel = jnp.concatenate([top_idx, own_b], axis=-1)
    sel_valid = jnp.concatenate([top_valid, jnp.ones((b, h, s_pad, 1), dtype=bool)], axis=-1)
    nsel = ksel + 1
    qc = MOBA_QUERY_CHUNK
    nq = s_pad // qc
    scale = hd ** -0.5
    bi = jnp.arange(b)[:, None, None, None]
    hi = jnp.arange(h)[None, :, None, None]

    def chunkify(t):
        t = t.reshape((b, h, nq, qc) + t.shape[3:])
        return jnp.moveaxis(t, 2, 0)

    def step(args):
        q_c, sel_c, val_c, pos_c = args
        kg = kb[bi, hi, sel_c]
        vg = vb[bi, hi, sel_c]
        sc = jnp.einsum('bhqd,bhqjkd->bhqjk', q_c, kg,
                        preferred_element_type=jnp.float32) * scale
        key_pos = sel_c[..., None] * bs + jnp.arange(bs)
        dist = pos_c[None, None, :, None, None] - key_pos
        mask = val_c[..., None] & (dist >= 0)
        sc = jnp.where(mask, sc - slopes[None, :, None, None, None] * dist.astype(jnp.float32), -jnp.inf)
        pr = jax.nn.softmax(sc.reshape(b, h, qc, nsel * bs), axis=-1).reshape(b, h, qc, nsel, bs)
        return jnp.einsum('bhqjk,bhqjkd->bhqd', pr, vg, preferred_element_type=jnp.float32)

    out = lax.map(step, (chunkify(q), chunkify(sel), chunkify(sel_valid), pos.reshape(nq, qc)))
    out = jnp.moveaxis(out, 0, 2).reshape(b, h, s_pad, hd)
    return out[:, :, :s]


def split_heads(t, nh):
    b, s, _ = t.shape
    t = t.reshape(b, s, 3, nh, HEAD_DIM).transpose(2, 0, 3, 1, 4)
    return t[0], t[1], t[2]


def merge_heads(t):
    b, h, s, hd = t.shape
    return t.transpose(0, 2, 1, 3).reshape(b, s, h * hd)


def setup_inputs(seed: int = 0) -> dict:
    key = jax.random.key(seed)
    ks = jax.random.split(key, 16)
    f32 = jnp.float32

    def nrm(k, shape, fan_in):
        return jax.random.normal(k, shape, f32) * fan_in ** -0.5

    def gain(k, shape):
        return 1.0 + 0.02 * jax.random.normal(k, shape, f32)

    return {
        'x': jax.random.normal(ks[0], (BATCH, SEQ, D_MODEL), f32),
        'p': jax.random.normal(ks[1], (DEPTH, BATCH, SEQ, PLE_DIM), f32),
        'g_attn': gain(ks[2], (DEPTH, D_MODEL)),
        'w_in': nrm(ks[3], (DEPTH, D_MODEL, 3 * D_MODEL), D_MODEL),
        'g_out_a': gain(ks[4], (DEPTH, WIDTH_A)),
        'g_out_b': gain(ks[5], (DEPTH, WIDTH_B)),
        'w_out': nrm(ks[6], (DEPTH, D_MODEL, D_MODEL), D_MODEL),
        'g_mlp': gain(ks[7], (DEPTH, D_MODEL)),
        'w_up': nrm(ks[8], (DEPTH, D_MODEL, D_FF), D_MODEL),
        'w_down': nrm(ks[9], (DEPTH, D_FF, D_MODEL), D_FF),
        'g_ple': gain(ks[10], (DEPTH, D_MODEL)),
        'w_ple_gate': nrm(ks[11], (DEPTH, D_MODEL, D_MODEL), D_MODEL),
        'b_ple_gate': 0.02 * jax.random.normal(ks[12], (DEPTH, D_MODEL), f32),
        'w_ple_proj': nrm(ks[13], (DEPTH, PLE_DIM, D_MODEL), PLE_DIM),
        'g_final': gain(ks[14], (D_MODEL,)),
    }


def reference(x, p, g_attn, w_in, g_out_a, g_out_b, w_out, g_mlp, w_up, w_down,
              g_ple, w_ple_gate, b_ple_gate, w_ple_proj, g_final):
    slopes = alibi_slopes(N_HEADS)
    slopes_a = slopes[0::2]
    slopes_b = slopes[1::2]
    h = x
    for i in range(DEPTH):
        hn = rmsnorm(h, g_attn[i])
        qkv = hn @ w_in[i]
        qa, ka, va = split_heads(qkv[..., :3 * WIDTH_A], N_HEADS_A)
        qb, kb, vb = split_heads(qkv[..., 3 * WIDTH_A:], N_HEADS_B)
        ya = rmsnorm(merge_heads(dilated_mixture(qa, ka, va, slopes_a)), g_out_a[i])
        yb = rmsnorm(merge_heads(moba_attention(qb, kb, vb, slopes_b)), g_out_b[i])
        y = jnp.concatenate([ya, yb], axis=-1).astype(h.dtype) @ w_out[i]
        h = h + y
        hn = rmsnorm(h, g_mlp[i])
        h = h + jnp.square(jax.nn.relu(hn @ w_up[i])) @ w_down[i]
        gate = jax.nn.sigmoid(rmsnorm(h, g_ple[i]) @ w_ple_gate[i] + b_ple_gate[i])
        h = h + gate * (p[i] @ w_ple_proj[i])
    return rmsnorm(h, g_final)
```

```python
import numpy as np
import concourse.bass as bass
import concourse.mybir as mybir
from concourse.bass_utils import run_bass_kernel_spmd

F32 = mybir.dt.float32
BF16 = mybir.dt.bfloat16
U8 = mybir.dt.uint8
ALU = mybir.AluOpType
AF = mybir.ActivationFunctionType
AX = mybir.AxisListType

S = 4096
D = 1024
NT = S // 128
DFF = 4096
PLE = 256
EPS = 1e-6
SLOPE_A = [2.0 ** (-(h + 0.5)) for h in range(8)]
SLOPE_B = [2.0 ** (-(h + 1.0)) for h in range(8)]
DILS = (1, 4, 16)
ENGS = ("pe", "act", "dve", "pool", "sp")


class Op:
    __slots__ = ("eng", "fn", "deps", "signal", "is_dma", "key", "val", "idx")


class Prog:
    def __init__(self):
        self.ops = {e: [] for e in ENGS}
        self.lastw = {}
        self.readers = {}
        self.dma_last = {}
        self.dma_count = {}

    def op(self, eng, fn, reads=(), writes=(), after=(), dma=None):
        o = Op()
        o.eng = eng
        o.fn = fn
        o.signal = False
        o.is_dma = dma is not None
        o.key = dma
        o.val = 0
        ps_reads = [b for b in reads if isinstance(b, tuple) and b[0] == "ps"]
        if ps_reads:
            reads = [b for b in reads if b not in ps_reads]
            writes = list(writes) + ps_reads
        cand = []
        for b in reads:
            w = self.lastw.get(b)
            if w is not None:
                cand.append(w)
        for b in writes:
            w = self.lastw.get(b)
            if w is not None:
                cand.append(w)
            cand.extend(self.readers.get(b, ()))
        cand.extend(after)
        if dma is not None:
            p = self.dma_last.get(dma)
            if p is not None:
                cand.append(p)
            self.dma_last[dma] = o
            self.dma_count[dma] = self.dma_count.get(dma, 0) + 1
            o.val = 16 * self.dma_count[dma]
            o.signal = True
        best = {}
        for d in cand:
            if d is o:
                continue
            if d.is_dma:
                k = ("dma", d.key)
                if k not in best or best[k].val < d.val:
                    best[k] = d
            else:
                if d.eng == "pe" and eng == "pe" and dma is None:
                    continue
                k = ("eng", d.eng)
                if k not in best or best[k].idx < d.idx:
                    best[k] = d
        o.deps = list(best.values())
        for d in o.deps:
            d.signal = True
        for b in reads:
            self.readers.setdefault(b, []).append(o)
        for b in writes:
            self.lastw[b] = o
            self.readers[b] = []
        o.idx = len(self.ops[eng])
        self.ops[eng].append(o)
        return o

    def barrier(self):
        lasts = []
        for e in ENGS:
            for o in reversed(self.ops[e]):
                if o.fn is not None and not o.is_dma:
                    lasts.append(o)
                    break
        lasts.extend(self.dma_last.values())
        for e in ENGS:
            self.op(e, None, after=lasts)
        self.lastw = {}
        self.readers = {}

    def finalize(self):
        for e in ENGS:
            n = 0
            for o in self.ops[e]:
                if o.is_dma:
                    continue
                if o.signal and o.fn is not None:
                    n += 1
                    o.val = n

    def emit(self, e, eng, esems, dsems):
        known = {}
        for o in self.ops[e]:
            for d in o.deps:
                if d.is_dma:
                    k = ("dma", d.key)
                    sem = dsems[d.key]
                else:
                    k = ("eng", d.eng)
                    sem = esems[d.eng]
                if known.get(k, 0) >= d.val:
                    continue
                eng.wait_ge(sem, d.val)
                known[k] = d.val
            if o.fn is None:
                continue
            last = o.fn(eng)
            if o.signal:
                if o.is_dma:
                    last.then_inc(dsems[o.key], 16)
                else:
                    last.then_inc(esems[e], 1)


class Arena:
    def __init__(self, nc, nbytes):
        self.t = nc.alloc_sbuf_tensor("arena", [128, nbytes], U8)
        self.n = nbytes

    def view(self, off, shape, dt):
        sz = 4 if dt == F32 else 2
        n = int(np.prod(shape[1:]))
        assert off % 4 == 0 and off + n * sz <= self.n, (off, shape, self.n)
        v = self.t[:, off:off + n * sz].bitcast(dt)
        if len(shape) > 2:
            names = " ".join("d%d" % i for i in range(1, len(shape)))
            kw = {"d%d" % i: shape[i] for i in range(1, len(shape))}
            v = v.rearrange("p (%s) -> p %s" % (names, names), **kw)
        return v


class Lay:
    def __init__(self, arena, start, limit):
        self.a = arena
        self.o = start
        self.limit = limit

    def get(self, shape, dt):
        sz = 4 if dt == F32 else 2
        n = int(np.prod(shape[1:])) * sz
        n = (n + 31) // 32 * 32
        v = self.a.view(self.o, shape, dt)
        self.o += n
        assert self.o <= self.limit, (self.o, self.limit)
        return v


def build_nc(stage=99, opts=None):
    opts = opts or {}
    nc = bass.Bass("TRN2", target_bir_lowering=False)
    P = Prog()

    def dram_in(name, shape):
        return nc.dram_tensor(name, shape, F32, kind="ExternalInput").ap()

    x = dram_in("x", [S, D])
    pin = dram_in("p", [S, PLE])
    g_attn = dram_in("g_attn", [1, D])
    w_in = dram_in("w_in", [D, 3 * D])
    g_out = dram_in("g_out", [1, D])
    w_out = dram_in("w_out", [D, D])
    g_mlp = dram_in("g_mlp", [1, D])
    w_up = dram_in("w_up", [D, DFF])
    w_down = dram_in("w_down", [DFF, D])
    g_ple = dram_in("g_ple", [1, D])
    w_gate = dram_in("w_gate", [D, D])
    b_gate = dram_in("b_gate", [1, D])
    w_proj = dram_in("w_proj", [PLE, D])
    g_final = dram_in("g_final", [1, D])
    out = nc.dram_tensor("out", [S, D], F32, kind="ExternalOutput").ap()
    skind = "ExternalOutput" if stage < 99 else "Internal"
    uscr = nc.dram_tensor("uscr", [4, S, 520], F32, kind=skind).ap()
    h2scr = nc.dram_tensor("h2scr", [S, D], F32, kind=skind).ap()

    ARENA = 212000
    ar = Arena(nc, ARENA)
    psall = nc.alloc_psum_tensor("psall", [128, 4096], F32)

    def psf(i):
        return psall[:, i * 512:(i + 1) * 512]

    def psb(i):
        return psall[:, i * 512:(i + 1) * 512].bitcast(BF16)

    L = Lay(ar, 0, 12288)
    ident = L.get([128, 128], BF16)
    epsT = L.get([128, 1], F32)
    oneT = L.get([128, 1], F32)
    gA = L.get([128, 8], F32)
    gM = L.get([128, 8], F32)
    gP = L.get([128, 8], F32)
    gO = L.get([128, 8], F32)
    CONST_KEEP = L.o
    D0 = L.get([128, 128], F32)
    Dpos = L.get([128, 128], F32)
    Dneg = L.get([128, 128], F32)
    D128 = L.get([128, 128], F32)
    Mcur = L.get([128, 128], F32)
    Mprev = L.get([128, 128], F32)
    facv = L.get([128, 2], F32)
    fac = L.get([128, 8, 2], F32)
    vgm = L.get([128, 32, 16], F32)
    negm = L.get([128, 32, 16], F32)
    CONST_END = L.o

    def cop(eng, fn, reads=(), writes=()):
        return P.op(eng, fn, reads, writes)

    cop("pool", lambda g: g.iota(D0, [[1, 128]], base=0, channel_multiplier=-1,
                                 allow_small_or_imprecise_dtypes=True), writes=["D0"])
    cop("pool", lambda g: g.iota(facv, [[128, 2]], base=-255, channel_multiplier=1,
                                 allow_small_or_imprecise_dtypes=True), writes=["facv"])
    cop("pool", lambda g: g.iota(vgm, [[128, 32], [-256, 16]], base=-255, channel_multiplier=1,
                                 allow_small_or_imprecise_dtypes=True), writes=["vgm0"])
    cop("pool", lambda g: g.iota(negm, [[1, 16], [0, 2], [-1, 16]], base=0, channel_multiplier=0,
                                 allow_small_or_imprecise_dtypes=True), writes=["negm0"])
    cop("dve", lambda v: v.memset(epsT, EPS), writes=["eps"])
    cop("dve", lambda v: v.memset(oneT, 1.0), writes=["one"])
    cop("dve", lambda v: v.tensor_scalar(ident, D0, 0.0, None, ALU.is_equal), reads=["D0"], writes=["ident"])
    cop("dve", lambda v: v.tensor_scalar(Dpos, D0, 0.0, None, ALU.max), reads=["D0"], writes=["Dpos"])
    cop("dve", lambda v: v.tensor_scalar(Dneg, D0, 0.0, 128.0, ALU.min, ALU.add), reads=["D0"], writes=["Dneg"])
    cop("dve", lambda v: v.tensor_scalar(D128, D0, 128.0, None, ALU.add), reads=["D0"], writes=["D128"])
    cop("dve", lambda v: v.tensor_scalar(Mcur, D0, 0.0, None, ALU.is_ge), reads=["D0"], writes=["Mcur"])
    cop("dve", lambda v: v.tensor_scalar(Mprev, D0, 0.0, None, ALU.is_le), reads=["D0"], writes=["Mprev"])
    cop("dve", lambda v: v.tensor_scalar(vgm, vgm, 0.0, None, ALU.max), reads=["vgm0"], writes=["vgm"])
    cop("dve", lambda v: v.tensor_scalar(negm, negm, 0.5, -1e30, ALU.is_lt, ALU.mult), reads=["negm0"], writes=["negm"])
    for h in range(8):
        cop("act", lambda a, h=h: a.activation(fac[:, h, :], facv, AF.Exp, scale=SLOPE_B[h]),
            reads=["facv"], writes=[("fac", h)])

    def gload(dst, src, key):
        P.op("sp", lambda q: q.dma_start(out=dst, in_=src.rearrange("o (c p) -> p (o c)", p=128),
                                         allow_slow_non_contiguous=True),
             writes=[key], dma=("g", key))

    gload(gA, g_attn, "gA")
    gload(gM, g_mlp, "gM")
    gload(gP, g_ple, "gP")
    gload(gO, g_out, "gO")

    dbg = {}

    L = Lay(ar, CONST_END, ARENA)
    hnT = L.get([128, 8, S], BF16)
    A0 = L.o
    xt = [L.get([128, D], F32) for _ in range(2)]
    hnb = [L.get([128, D], BF16) for _ in range(2)]
    ssq1 = L.get([128, NT], F32)
    rs1 = L.get([128, NT], F32)
    junk = L.get([128, D], F32)

    for T in range(NT):
        sl = T % 2
        P.op("sp", lambda q, T=T, sl=sl: q.dma_start(out=xt[sl], in_=x[T * 128:(T + 1) * 128, :]),
             writes=[("xt", sl)], dma=("xt", sl))
        P.op("dve", lambda v, T=T, sl=sl: v.scalar_tensor_tensor(
            out=junk, in0=xt[sl], scalar=1.0, in1=xt[sl], op0=ALU.mult, op1=ALU.mult,
            accum_out=ssq1[:, T:T + 1]), reads=[("xt", sl)], writes=["junk", ("ssq1", T)])
        P.op("act", lambda a, T=T: a.activation(rs1[:, T:T + 1], ssq1[:, T:T + 1], AF.Ln, bias=epsT, scale=1.0 / D),
             reads=[("ssq1", T), "eps"], writes=[("rs1", T)])
        P.op("act", lambda a, T=T: a.activation(rs1[:, T:T + 1], rs1[:, T:T + 1], AF.Exp, scale=-0.5),
             reads=[("rs1", T)], writes=[("rs1", T)])
        P.op("dve", lambda v, T=T, sl=sl: v.tensor_scalar(hnb[sl], xt[sl], rs1[:, T:T + 1], None, ALU.mult),
             reads=[("xt", sl), ("rs1", T)], writes=[("hnb", sl)])
        bank = T % 2

        def tr(pe, sl=sl, bank=bank):
            last = None
            for c in range(8):
                last = pe.transpose(psb(bank)[:, c * 128:(c + 1) * 128], hnb[sl][:, c * 128:(c + 1) * 128], ident)
            return last
        P.op("pe", tr, reads=[("hnb", sl), "ident"], writes=[("ps", bank)])
        src = psb(bank).rearrange("p (c t) -> p c t", c=8)
        dst = hnT[:, :, T * 128:(T + 1) * 128]
        if T % 2 == 0:
            P.op("act", lambda a, src=src, dst=dst: a.copy(dst, src), reads=[("ps", bank)], writes=[("hnT", T // 4)])
        else:
            P.op("dve", lambda v, src=src, dst=dst: v.tensor_copy(dst, src), reads=[("ps", bank)], writes=[("hnT", T // 4)])

    if stage == 1:
        dbg["hnT"] = (hnT, [128, 8, S], BF16)

    L = Lay(ar, A0, ARENA)
    wst = [L.get([128, 8, 384], F32) for _ in range(2)]
    wpr = [L.get([128, 8, 384], BF16) for _ in range(2)]
    QT = L.get([128, S], BF16)
    KT = L.get([128, S], BF16)
    VT = L.get([128, S], BF16)
    Vaug = L.get([128, 3, 32, 2, 65], BF16)
    PT = [L.get([128, 512], BF16) for _ in range(3)]
    WA = L.get([128, 4, 3, 512], BF16)
    WD = L.get([128, 8, 384], BF16)
    Uev = [L.get([128, 130], F32) for _ in range(4)]
    etmp = [L.get([128, 128], F32) for _ in range(2)]
    ksum = L.get([128, 16], F32)
    kmT = L.get([128, 16], BF16)
    gm = L.get([128, 2, 32, 16], F32)
    t8 = L.get([128, 2, 32, 8], F32)
    cT = L.get([128, 2, 32, 16], F32)
    accB = [L.get([128, 2, 2, 65], F32) for _ in range(2)]

    if stage >= 2:
        P.barrier()
        P.op("pool", lambda g: g.memset(Vaug[:, :, :, :, 64:65], 1.0), writes=["vones"])
        k = 0
        for pr in range(4):
            for bi, dil in enumerate(DILS):
                for hh in range(2):
                    sl_ = SLOPE_A[2 * pr + hh]
                    for which, (dsrc, msk, dkey, mkey) in enumerate(((Dpos, Mcur, "Dpos", "Mcur"), (Dneg, Mprev, "Dneg", "Mprev"))):
                        e = k % 2
                        k += 1
                        col = hh * 256 + which * 128
                        P.op("act", lambda a, e=e, dsrc=dsrc, sc=-sl_ * dil: a.activation(etmp[e], dsrc, AF.Exp, scale=sc),
                             reads=[dkey], writes=[("etmp", e)])
                        P.op("dve", lambda v, e=e, msk=msk, pr=pr, bi=bi, col=col: v.tensor_tensor(
                            WA[:, pr, bi, col:col + 128], etmp[e], msk, ALU.mult),
                            reads=[("etmp", e), mkey], writes=["WA"])
        for h in range(8):
            e = k % 2
            k += 1
            P.op("act", lambda a, e=e, h=h: a.activation(etmp[e], Dpos, AF.Exp, scale=-SLOPE_B[h]),
                 reads=["Dpos"], writes=[("etmp", e)])
            P.op("dve", lambda v, e=e, h=h: v.tensor_tensor(WD[:, h, 0:128], etmp[e], Mcur, ALU.mult),
                 reads=[("etmp", e), "Mcur"], writes=["WD"])
            P.op("dve", lambda v, e=e, h=h: v.tensor_tensor(WD[:, h, 256:384], etmp[e], Mcur, ALU.mult),
                 reads=[("etmp", e), "Mcur"], writes=["WD"])
            P.op("act", lambda a, h=h: a.activation(WD[:, h, 128:256], D128, AF.Exp, scale=-SLOPE_B[h]),
                 reads=["D128"], writes=["WD"])

    rot = {"proj": 0, "S": 0, "O": 0, "PT": 0, "U": 0, "ev": 0}

    def nxt(name, n):
        v = rot[name]
        rot[name] = (v + 1) % n
        return v

    def load_wpair(pi):
        slot = pi % 2
        grp, pr = divmod(pi, 4)
        base = grp * 1536 + pr * 128
        for w in range(3):
            c0 = base + w * 512
            P.op("sp", lambda q, slot=slot, w=w, c0=c0: q.dma_start(
                out=wst[slot][:, :, w * 128:(w + 1) * 128],
                in_=w_in[:, c0:c0 + 128].rearrange("(c p) n -> p c n", p=128)),
                writes=[("wst", slot, w)], dma=("wst", slot, w))
            eng = ("dve", "pool", "dve")[w]
            P.op(eng, lambda v, slot=slot, w=w: v.tensor_tensor(
                wpr[slot][:, :, w * 128:(w + 1) * 128], wst[slot][:, :, w * 128:(w + 1) * 128],
                gA.unsqueeze(2).to_broadcast([128, 8, 128]), ALU.mult),
                reads=[("wst", slot, w), "gA"], writes=[("wpr", slot, w)])

    def project(pi, is_b):
        slot = pi % 2
        for w, dst, key in ((0, QT, "QT"), (1, KT, "KT"), (2, VT, "VT")):
            for tc in range(8):
                bank = nxt("proj", 2)

                def mm(pe, slot=slot, w=w, tc=tc, bank=bank):
                    last = None
                    for kc in range(8):
                        last = pe.matmul(psf(bank), wpr[slot][:, kc, w * 128:(w + 1) * 128],
                                         hnT[:, kc, tc * 512:(tc + 1) * 512], start=(kc == 0), stop=(kc == 7))
                    return last
                P.op("pe", mm, reads=[("wpr", slot, w), ("hnT", tc)], writes=[("ps", bank)])
                d = dst[:, tc * 512:(tc + 1) * 512]
                if tc % 2 == 0:
                    P.op("act", lambda a, d=d, bank=bank: a.copy(d, psf(bank)), reads=[("ps", bank)], writes=[(key, tc)])
                else:
                    P.op("dve", lambda v, d=d, bank=bank: v.tensor_copy(d, psf(bank)), reads=[("ps", bank)], writes=[(key, tc)])
                if is_b and w == 1:
                    P.op("dve", lambda v, tc=tc, bank=bank: v.tensor_reduce(
                        ksum[:, tc * 2:(tc + 1) * 2], psf(bank).rearrange("p (a b) -> p a b", a=2), AX.X, ALU.add),
                        reads=[("ps", bank)], writes=[("ksum", tc)])

    def tok_ap(t, dil, r, n):
        st = r + 128 * dil * n
        return t[:, st:st + 127 * dil + 1:dil] if dil > 1 else t[:, st:st + 128]

    def blocks(dil):
        nb = 32 // dil
        return [(r, n) for r in range(dil) for n in range(nb)]

    def build_vaug(ords):
        for oi, dil in ords:
            bl = blocks(dil)
            for g0 in range(0, 32, 8):
                bank = nxt("ev", 2)

                def tr(pe, dil=dil, g0=g0, bank=bank, bl=bl):
                    last = None
                    for j in range(8):
                        r, n = bl[g0 + j]
                        last = pe.transpose(psb(bank)[:, j * 128:(j + 1) * 128], tok_ap(VT, dil, r, n), ident)
                    return last
                P.op("pe", tr, reads=[("VT", i) for i in range(8)] + ["ident"], writes=[("ps", bank)])
                src = psb(bank).rearrange("p (j h d) -> p j h d", j=8, h=2)
                dst = Vaug[:, oi, g0:g0 + 8, :, 0:64]
                if (g0 // 8) % 2 == 0:
                    P.op("dve", lambda v, src=src, dst=dst: v.tensor_copy(dst, src),
                         reads=[("ps", bank), "vones"], writes=[("Vaug", oi)])
                else:
                    P.op("act", lambda a, src=src, dst=dst: a.copy(dst, src),
                         reads=[("ps", bank), "vones"], writes=[("Vaug", oi)])

    allkeys = lambda name: [(name, i) for i in range(8)]

    def pipeline(iters, depth=2):
        n = len(iters)
        for i in range(n + depth):
            if i < n:
                iters[i][0]()
            if i - depth >= 0:
                iters[i - depth][1]()

    def attn_dilated(pr):
        iters = []
        for bi, dil in enumerate(DILS):
            bl = blocks(dil)
            bidx = {b: i for i, b in enumerate(bl)}
            for (r, n) in bl:
                ss = nxt("S", 2)
                sb0 = 2 + 2 * ss
                ob = 6 + nxt("O", 2)
                pt = nxt("PT", 3)
                us = nxt("U", 4)
                ncol = 256 if n > 0 else 128

                def stA(bi=bi, dil=dil, r=r, n=n, sb0=sb0, pt=pt, ncol=ncol):
                    def sc(pe):
                        last = None
                        for hh in range(2):
                            rows = slice(hh * 64, hh * 64 + 64)
                            qa = tok_ap(QT, dil, r, n)[rows, :]
                            last = pe.matmul(psf(sb0 + hh)[:, 0:128], tok_ap(KT, dil, r, n)[rows, :], qa,
                                             start=True, stop=True)
                            if n > 0:
                                last = pe.matmul(psf(sb0 + hh)[:, 128:256],
                                                 tok_ap(KT, dil, r, n - 1)[rows, :], qa, start=True, stop=True)
                        return last
                    P.op("pe", sc, reads=allkeys("QT") + allkeys("KT"), writes=[("ps", sb0), ("ps", sb0 + 1)])
                    src = psall[:, sb0 * 512:(sb0 + 2) * 512].rearrange("p (h c) -> p h c", h=2)[:, :, 0:ncol]
                    ptv = PT[pt].rearrange("p (h c) -> p h c", h=2)[:, :, 0:ncol]
                    wav = WA[:, pr, bi, :].rearrange("p (h c) -> p h c", h=2)[:, :, 0:ncol]
                    P.op("act", lambda a: a.activation(ptv, src, AF.Exp, scale=0.125),
                         reads=[("ps", sb0), ("ps", sb0 + 1)], writes=[("PT", pt)])
                    P.op("dve", lambda v: v.tensor_tensor(ptv, ptv, wav, ALU.mult),
                         reads=[("PT", pt), "WA"], writes=[("PT", pt)])

                def stB(bi=bi, dil=dil, r=r, n=n, ob=ob, pt=pt, us=us, bidx=bidx):
                    def pv(pe):
                        last = None
                        for hh in range(2):
                            o = psf(ob)[:, hh * 65:(hh + 1) * 65]
                            last = pe.matmul(o, PT[pt][:, hh * 256:hh * 256 + 128], Vaug[:, bi, bidx[(r, n)], hh, :],
                                             start=True, stop=(n == 0))
                            if n > 0:
                                last = pe.matmul(o, PT[pt][:, hh * 256 + 128:hh * 256 + 256],
                                                 Vaug[:, bi, bidx[(r, n - 1)], hh, :], start=False, stop=True)
                        return last
                    P.op("pe", pv, reads=[("PT", pt), ("Vaug", bi)], writes=[("ps", ob)])
                    P.op("dve", lambda v: v.tensor_copy(Uev[us], psf(ob)[:, 0:130]),
                         reads=[("ps", ob)], writes=[("Uev", us)])
                    st = r + 128 * dil * n
                    dst = uscr[bi, st:st + 127 * dil + 1:dil, pr * 130:(pr + 1) * 130] if dil > 1 else \
                        uscr[bi, st:st + 128, pr * 130:(pr + 1) * 130]
                    P.op("pool", lambda g: g.dma_start(out=dst, in_=Uev[us]),
                         reads=[("Uev", us)], dma=("Uev", us))
                iters.append((stA, stB))
        pipeline(iters)

    def attn_moba(pr):
        for hh in range(2):
            h = 2 * pr + hh
            for j in range(2):
                eng = "pool" if j == 0 else "dve"
                P.op(eng, lambda v, hh=hh, h=h, j=j: v.tensor_scalar(
                    Vaug[:, 1, j:32:2, hh, :], Vaug[:, 0, j:32:2, hh, :], fac[:, h, j:j + 1], None, ALU.mult),
                    reads=[("Vaug", 0), ("fac", h), "vones"], writes=[("Vaug", 1)])
        P.op("dve", lambda v: v.tensor_scalar(kmT, ksum, 1.0 / 256.0, None, ALU.mult),
             reads=[("ksum", i) for i in range(8)], writes=["kmT"])
        for hh in range(2):
            h = 2 * pr + hh
            rows = slice(hh * 64, hh * 64 + 64)
            gb = hh

            def gmm(pe, rows=rows, gb=gb):
                last = None
                for qt in range(32):
                    last = pe.matmul(psf(gb)[:, qt * 16:(qt + 1) * 16], QT[rows, qt * 128:(qt + 1) * 128], kmT[rows, :],
                                     start=True, stop=True)
                return last
            P.op("pe", gmm, reads=allkeys("QT") + ["kmT"], writes=[("ps", gb)])
            P.op("dve", lambda v, hh=hh, gb=gb: v.tensor_tensor(
                gm[:, hh], psf(gb).rearrange("p (a b) -> p a b", a=32), negm, ALU.add),
                reads=[("ps", gb), "negm"], writes=[("gm", hh)])

            def mx(v, hh=hh):
                last = None
                for qt in range(32):
                    last = v.max(t8[:, hh, qt, :], gm[:, hh, qt, :])
                return last
            P.op("dve", mx, reads=[("gm", hh)], writes=[("t8", hh)])
            P.op("act", lambda a, hh=hh, h=h: a.activation(cT[:, hh], vgm, AF.Exp, scale=-SLOPE_B[h]),
                 reads=["vgm"], writes=[("cT", hh)])
            P.op("dve", lambda v, hh=hh: v.tensor_tensor(
                gm[:, hh], gm[:, hh], t8[:, hh, :, 2:3].to_broadcast([128, 32, 16]), ALU.is_ge),
                reads=[("gm", hh), ("t8", hh)], writes=[("gm", hh)])
            P.op("dve", lambda v, hh=hh: v.tensor_tensor(cT[:, hh], cT[:, hh], gm[:, hh], ALU.mult),
                 reads=[("gm", hh), ("cT", hh)], writes=[("cT", hh)])
        iters = []
        nbq = opts.get('moba_nb', 16)
        for b in range(nbq):
            ab = b % 2
            for hh in range(2):
                h = 2 * pr + hh
                rows = slice(hh * 64, hh * 64 + 64)
                sb = 2 + nxt("S", 4)
                ob = 6 + nxt("O", 2)
                pt = nxt("PT", 3)

                def dA(b=b, rows=rows, sb=sb, pt=pt, h=h):
                    def scd(pe):
                        pe.matmul(psf(sb)[:, 0:256], KT[rows, b * 256:b * 256 + 128], QT[rows, b * 256:(b + 1) * 256],
                                  start=True, stop=True)
                        return pe.matmul(psf(sb)[:, 256:384], KT[rows, b * 256 + 128:(b + 1) * 256],
                                         QT[rows, b * 256 + 128:(b + 1) * 256], start=True, stop=True)
                    P.op("pe", scd, reads=allkeys("QT") + allkeys("KT"), writes=[("ps", sb)])
                    P.op("act", lambda a: a.activation(PT[pt][:, 0:384], psf(sb)[:, 0:384], AF.Exp, scale=0.125),
                         reads=[("ps", sb)], writes=[("PT", pt)])
                    P.op("dve", lambda v: v.tensor_tensor(PT[pt][:, 0:384], PT[pt][:, 0:384], WD[:, h, :], ALU.mult),
                         reads=[("PT", pt), "WD"], writes=[("PT", pt)])

                def dB(b=b, hh=hh, ob=ob, pt=pt, ab=ab):
                    def pvd(pe):
                        pe.matmul(psf(ob)[:, 0:65], PT[pt][:, 0:128], Vaug[:, 0, 2 * b, hh, :], start=True, stop=True)
                        pe.matmul(psf(ob)[:, 65:130], PT[pt][:, 128:256], Vaug[:, 0, 2 * b, hh, :], start=True, stop=False)
                        return pe.matmul(psf(ob)[:, 65:130], PT[pt][:, 256:384], Vaug[:, 0, 2 * b + 1, hh, :], start=False, stop=True)
                    P.op("pe", pvd, reads=[("PT", pt), ("Vaug", 0)], writes=[("ps", ob)])
                    P.op("dve", lambda v: v.tensor_copy(
                        accB[ab][:, :, hh, :], psf(ob)[:, 0:130].rearrange("p (j d) -> p j d", j=2)),
                        reads=[("ps", ob)], writes=[("acc", ab, hh)])
                iters.append((dA, dB))
                for n in range(b):
                    sb = 2 + nxt("S", 4)
                    ob = 6 + nxt("O", 2)
                    pt = nxt("PT", 3)

                    def oA(b=b, n=n, rows=rows, sb=sb, pt=pt):
                        def sco(pe):
                            q = QT[rows, b * 256:(b + 1) * 256]
                            pe.matmul(psf(sb)[:, 0:256], KT[rows, n * 256:n * 256 + 128], q, start=True, stop=True)
                            return pe.matmul(psf(sb)[:, 256:512], KT[rows, n * 256 + 128:(n + 1) * 256], q, start=True, stop=True)
                        P.op("pe", sco, reads=allkeys("QT") + allkeys("KT"), writes=[("ps", sb)])
                        P.op("act", lambda a: a.activation(PT[pt], psf(sb), AF.Exp, scale=0.125),
                             reads=[("ps", sb)], writes=[("PT", pt)])

                    def oB(b=b, n=n, hh=hh, ob=ob, pt=pt, ab=ab):
                        def pvo(pe):
                            last = None
                            for j in range(2):
                                o = psf(ob)[:, j * 65:(j + 1) * 65]
                                pe.matmul(o, PT[pt][:, j * 128:(j + 1) * 128], Vaug[:, 1, 2 * n, hh, :], start=True, stop=False)
                                last = pe.matmul(o, PT[pt][:, 256 + j * 128:256 + (j + 1) * 128], Vaug[:, 1, 2 * n + 1, hh, :],
                                                 start=False, stop=True)
                            return last
                        P.op("pe", pvo, reads=[("PT", pt), ("Vaug", 1)], writes=[("ps", ob)])

                        def accf(v):
                            last = None
                            for j in range(2):
                                last = v.scalar_tensor_tensor(
                                    out=accB[ab][:, j, hh, :], in0=psf(ob)[:, j * 65:(j + 1) * 65],
                                    scalar=cT[:, hh, 2 * b + j, n:n + 1], in1=accB[ab][:, j, hh, :],
                                    op0=ALU.mult, op1=ALU.add)
                            return last
                        P.op("dve", accf, reads=[("ps", ob), ("cT", hh), ("acc", ab, hh)], writes=[("acc", ab, hh)])
                    iters.append((oA, oB))

            def stA(): pass

            def stB(b=b, ab=ab):
                dst = uscr[3, b * 256:(b + 1) * 256, pr * 130:(pr + 1) * 130]
                P.op("pool", lambda g: g.dma_start(
                    out=dst.rearrange("(j p) c -> p j c", p=128), in_=accB[ab].rearrange("p j h d -> p j (h d)")),
                    reads=[("acc", ab, 0), ("acc", ab, 1)], dma=("acc", ab))
            iters.append((stA, stB))
        pipeline(iters)

    if stage >= 2:
        plist = opts.get('pairs', {2: [0], 3: [0, 1, 2, 3]}.get(stage, list(range(8))))
        load_wpair(plist[0])
        for ii, pi in enumerate(plist):
            if ii + 1 < len(plist):
                load_wpair(plist[ii + 1])
            is_b = pi >= 4
            project(pi, is_b)
            if stage == 2:
                dbg["QT"] = (QT, [128, S], BF16)
                dbg["KT"] = (KT, [128, S], BF16)
                dbg["VT"] = (VT, [128, S], BF16)
            if not is_b:
                build_vaug([(0, 1), (1, 4), (2, 16)])
                attn_dilated(pi)
            else:
                build_vaug([(0, 1)])
                attn_moba(pi - 4)

    if stage >= 5 and opts.get('p3', True):
        P.barrier()
        L = Lay(ar, CONST_KEEP, ARENA)
        woT = L.get([128, 8, D], BF16)
        wuT = L.get([128, 8, DFF], BF16)
        wdT = L.get([128, 32, D], BF16)
        stg = [L.get([128, 512], F32) for _ in range(2)]
        Ut = L.get([128, 4, 520], F32)
        xh = [L.get([128, D], F32) for _ in range(4)]
        ynb = L.get([128, D], BF16)
        ynT = L.get([128, 8, 128], BF16)
        hn2T = L.get([128, 8, 256], BF16)
        actT = L.get([128, 32, 256], BF16)
        rtmp = [L.get([128, 512], F32) for _ in range(2)]
        sm = L.get([128, 16], F32)
        rec = L.get([128, 16], F32)
        cv = {"i": 0}

        def conv_w(dst3, src2, nk, ncols, gt, key, colmajor=False):
            step = 512
            order = [(kc, c0) for c0 in range(0, ncols, step) for kc in range(nk)] if colmajor else \
                [(kc, c0) for kc in range(nk) for c0 in range(0, ncols, step)]
            for kc, c0 in order:
                w = min(step, ncols - c0)
                s = cv["i"] % 2
                e = ("dve", "act")[cv["i"] % 2]
                cv["i"] += 1
                wkey = (key, c0 // step) if colmajor else (key, kc)
                P.op("sp", lambda q, s=s, kc=kc, c0=c0, w=w: q.dma_start(
                    out=stg[s][:, 0:w], in_=src2[kc * 128:(kc + 1) * 128, c0:c0 + w]),
                    writes=[("stg", s)], dma=("stg", s))
                d = dst3[:, kc, c0:c0 + w]
                if gt is None:
                    if e == "act":
                        P.op(e, lambda a, s=s, d=d, w=w: a.copy(d, stg[s][:, 0:w]), reads=[("stg", s)], writes=[wkey])
                    else:
                        P.op(e, lambda v, s=s, d=d, w=w: v.tensor_copy(d, stg[s][:, 0:w]), reads=[("stg", s)], writes=[wkey])
                else:
                    gk, gkey = gt
                    sc = gk[:, kc:kc + 1]
                    if e == "act":
                        P.op(e, lambda a, s=s, d=d, w=w, sc=sc: a.activation(d, stg[s][:, 0:w], AF.Copy, scale=sc),
                             reads=[("stg", s), gkey], writes=[wkey])
                    else:
                        P.op(e, lambda v, s=s, d=d, w=w, sc=sc: v.tensor_scalar(d, stg[s][:, 0:w], sc, None, ALU.mult),
                             reads=[("stg", s), gkey], writes=[wkey])

        conv_w(woT, w_out, 8, D, (gO, "gO"), "wo")
        conv_w(wuT, w_up, 8, DFF, (gM, "gM"), "wu", colmajor=True)
        conv_w(wdT, w_down, 32, D, None, "wd")

        def rstd_ops(col, scale):
            P.op("act", lambda a: a.activation(sm[:, col:col + 1], sm[:, col:col + 1], AF.Ln, bias=epsT, scale=scale),
                 reads=[("sm", col), "eps"], writes=[("sm", col)])
            P.op("act", lambda a: a.activation(sm[:, col:col + 1], sm[:, col:col + 1], AF.Exp, scale=-0.5),
                 reads=[("sm", col)], writes=[("sm", col)])

        def transposes8(src_bf, dst3, dkey, bank):
            def tr(pe):
                last = None
                for c in range(8):
                    last = pe.transpose(psb(bank)[:, c * 128:(c + 1) * 128], src_bf[:, c * 128:(c + 1) * 128], ident)
                return last
            return tr

        ngroups = opts.get('ngroups', 16 if stage >= 6 else 1)
        ynbA = [ynb[:, 0:D], L.get([128, D], BF16)]
        ynTs = [ynT, L.get([128, 8, 128], BF16)]
        hnb2 = ynbA
        TRB = (0, 7)

        def front_s0(G):
            for t in range(2):
                T = G * 2 + t
                xi = (G % 2) * 2 + t
                xs = xh[xi]
                P.op("sp", lambda q, T=T: q.dma_start(
                    out=Ut, in_=uscr[:, T * 128:(T + 1) * 128, :].rearrange("s p c -> p s c")),
                    writes=["Ut"], dma="Ut")
                P.op("sp", lambda q, T=T, xs=xs: q.dma_start(out=xs, in_=x[T * 128:(T + 1) * 128, :]),
                     writes=[("xh", xi)], dma=("xh", xi))
                P.op("dve", lambda v: v.tensor_tensor(Ut[:, 0], Ut[:, 0], Ut[:, 1], ALU.add), reads=["Ut"], writes=["Ut"])
                P.op("dve", lambda v: v.tensor_tensor(Ut[:, 0], Ut[:, 0], Ut[:, 2], ALU.add), reads=["Ut"], writes=["Ut"])
                jk = Ut[:, 1, 0:512]
                for gi, us in ((0, 0), (1, 3)):
                    Uv = Ut[:, us].rearrange("p (h d) -> p h d", h=8)
                    Ov = Uv[:, :, 0:64]
                    col = t * 2 + gi
                    P.op("dve", lambda v, gi=gi, Uv=Uv: v.reciprocal(rec[:, gi * 8:(gi + 1) * 8], Uv[:, :, 64]),
                         reads=["Ut"], writes=[("rec", gi)])
                    P.op("dve", lambda v, gi=gi, Ov=Ov: v.tensor_tensor(
                        Ov, Ov, rec[:, gi * 8:(gi + 1) * 8].unsqueeze(2).to_broadcast([128, 8, 64]), ALU.mult),
                        reads=["Ut", ("rec", gi)], writes=["Ut"])
                    P.op("dve", lambda v, col=col, Ov=Ov, jk=jk: v.scalar_tensor_tensor(
                        out=jk.rearrange("p (h d) -> p h d", h=8), in0=Ov, scalar=1.0,
                        in1=Ov, op0=ALU.mult, op1=ALU.mult, accum_out=sm[:, col:col + 1]),
                        reads=["Ut"], writes=["Ut", ("sm", col)])
                    rstd_ops(col, 1.0 / 512)
                    P.op("dve", lambda v, gi=gi, col=col, Ov=Ov, t=t: v.tensor_scalar(
                        ynbA[t][:, gi * 512:(gi + 1) * 512].rearrange("p (h d) -> p h d", h=8), Ov, sm[:, col:col + 1], None, ALU.mult),
                        reads=["Ut", ("sm", col)], writes=[("ynbA", t, gi)])

        def front_s1(G):
            for t in range(2):
                P.op("pe", transposes8(ynbA[t], None, None, TRB[t]), reads=[("ynbA", t, 0), ("ynbA", t, 1), "ident"], writes=[("ps", TRB[t])])
                P.op("act", lambda a, t=t: a.copy(ynTs[t], psb(TRB[t]).rearrange("p (c t) -> p c t", c=8)),
                     reads=[("ps", TRB[t])], writes=[("ynT", t)])
            for t in range(2):
                xi = (G % 2) * 2 + t
                xs = xh[xi]
                for half in range(2):
                    bank = 1 + half

                    def mo(pe, half=half, bank=bank, t=t):
                        last = None
                        for kc in range(8):
                            last = pe.matmul(psf(bank), ynTs[t][:, kc, :], woT[:, kc, half * 512:(half + 1) * 512],
                                             start=(kc == 0), stop=(kc == 7))
                        return last
                    P.op("pe", mo, reads=[("ynT", t)] + [("wo", kc) for kc in range(8)], writes=[("ps", bank)])
                    P.op("dve", lambda v, half=half, bank=bank, xs=xs: v.tensor_tensor(
                        xs[:, half * 512:(half + 1) * 512], xs[:, half * 512:(half + 1) * 512], psf(bank), ALU.add),
                        reads=[("ps", bank), ("xh", xi)], writes=[("xh", xi)])
                col = 4 + t
                P.op("dve", lambda v, xs=xs, t=t, col=col: v.scalar_tensor_tensor(
                    out=hnb2[t], in0=xs, scalar=1.0, in1=xs, op0=ALU.mult, op1=ALU.mult,
                    accum_out=sm[:, col:col + 1]),
                    reads=[("xh", xi)], writes=[("ynbA", t, 0), ("ynbA", t, 1), ("sm", col)])
                rstd_ops(col, 1.0 / D)
                P.op("dve", lambda v, xs=xs, t=t, col=col: v.tensor_scalar(hnb2[t], xs, sm[:, col:col + 1], None, ALU.mult),
                     reads=[("xh", xi), ("sm", col)], writes=[("ynbA", t, 0), ("ynbA", t, 1)])

        def front_s3(G):
            for t in range(2):
                P.op("pe", transposes8(hnb2[t], None, None, TRB[t]), reads=[("ynbA", t, 0), ("ynbA", t, 1), "ident"], writes=[("ps", TRB[t])])
                P.op("act", lambda a, t=t: a.copy(hn2T[:, :, t * 128:(t + 1) * 128], psb(TRB[t]).rearrange("p (c t) -> p c t", c=8)),
                     reads=[("ps", TRB[t])], writes=[("hn2T", t)])

        def up(G):
            for fp in range(16):
                bank = 3 + fp % 2

                def mu(pe, fp=fp, bank=bank):
                    last = None
                    for j in range(2):
                        ffc = fp * 2 + j
                        for kc in range(8):
                            last = pe.matmul(psf(bank)[:, j * 256:(j + 1) * 256], wuT[:, kc, ffc * 128:(ffc + 1) * 128],
                                             hn2T[:, kc, :], start=(kc == 0), stop=(kc == 7))
                    return last
                P.op("pe", mu, reads=[("hn2T", 0), ("hn2T", 1), ("wu", fp // 2)], writes=[("ps", bank)])
                rs = fp % 2
                P.op("act", lambda a, rs=rs, bank=bank: a.activation(rtmp[rs], psf(bank), AF.Relu),
                     reads=[("ps", bank)], writes=[("rtmp", rs)])
                e = "dve"
                P.op(e, lambda v, rs=rs, fp=fp: v.tensor_tensor(
                    actT[:, fp * 2:fp * 2 + 2, :], rtmp[rs].rearrange("p (j t) -> p j t", j=2),
                    rtmp[rs].rearrange("p (j t) -> p j t", j=2), ALU.mult),
                    reads=[("rtmp", rs)], writes=[("actT", fp)])

        def down(G):
            for t in range(2):
                T = G * 2 + t
                xi = (G % 2) * 2 + t
                xs = xh[xi]
                for half in range(2):
                    bank = 5 + half

                    def md(pe, t=t, half=half, bank=bank):
                        last = None
                        for ffc in range(32):
                            last = pe.matmul(psf(bank), actT[:, ffc, t * 128:(t + 1) * 128],
                                             wdT[:, ffc, half * 512:(half + 1) * 512], start=(ffc == 0), stop=(ffc == 31))
                        return last
                    P.op("pe", md, reads=[("actT", i) for i in range(16)] + [("wd", i) for i in range(32)], writes=[("ps", bank)])
                    P.op("dve", lambda v, half=half, bank=bank, xs=xs: v.tensor_tensor(
                        xs[:, half * 512:(half + 1) * 512], xs[:, half * 512:(half + 1) * 512], psf(bank), ALU.add),
                        reads=[("ps", bank), ("xh", xi)], writes=[("xh", xi)])
                P.op("pool", lambda g, T=T, xs=xs: g.dma_start(out=h2scr[T * 128:(T + 1) * 128, :], in_=xs),
                     reads=[("xh", xi)], dma=("h2s", xi))


        front_s0(0)
        front_s1(0)
        front_s3(0)
        for G in range(ngroups):
            if G + 1 < ngroups:
                front_s0(G + 1)
            up(G)
            if G + 1 < ngroups:
                front_s1(G + 1)
            down(G)
            if G + 1 < ngroups:
                front_s3(G + 1)
    if stage >= 7 and opts.get('p4', True):
        P.barrier()
        L = Lay(ar, CONST_KEEP, ARENA)
        wgT = L.get([128, 8, D], BF16)
        wpT = L.get([128, 2, D], BF16)
        stg4 = [L.get([128, 2048], F32) for _ in range(2)]
        bgb = L.get([128, D], F32)
        gfb = L.get([128, D], F32)
        h2 = [L.get([128, D], F32) for _ in range(2)]
        pt_ = [L.get([128, PLE], F32) for _ in range(2)]
        hb = L.get([128, D], BF16)
        pb = L.get([128, PLE], BF16)
        h3T = L.get([128, 8, 128], BF16)
        pT = L.get([128, 2, 128], BF16)
        gt = L.get([128, D], F32)
        junk4 = L.get([128, D], F32)
        sm4 = L.get([128, 4], F32)
        ot = [L.get([128, D], F32) for _ in range(2)]
        cv = {"i": 0}
        for kc in range(8):
            s = kc % 2
            P.op("sp", lambda q, s=s, kc=kc: q.dma_start(out=stg4[s][:, 0:D], in_=w_gate[kc * 128:(kc + 1) * 128, :]),
                 writes=[("stg4", s)], dma=("stg4", s))
            P.op("dve", lambda v, s=s, kc=kc: v.tensor_scalar(wgT[:, kc, :], stg4[s][:, 0:D], gP[:, kc:kc + 1], None, ALU.mult),
                 reads=[("stg4", s), "gP"], writes=[("wg", kc)])
        for kc in range(2):
            s = kc % 2
            P.op("sp", lambda q, s=s, kc=kc: q.dma_start(out=stg4[s][:, 0:D], in_=w_proj[kc * 128:(kc + 1) * 128, :]),
                 writes=[("stg4", s)], dma=("stg4", s))
            P.op("dve", lambda v, s=s, kc=kc: v.tensor_copy(wpT[:, kc, :], stg4[s][:, 0:D]),
                 reads=[("stg4", s)], writes=[("wp", kc)])
        P.op("sp", lambda q: q.dma_start(out=bgb, in_=b_gate.partition_broadcast(128)[:, 0, :]), writes=["bgb"], dma="bgb")
        P.op("sp", lambda q: q.dma_start(out=gfb, in_=g_final.partition_broadcast(128)[:, 0, :]), writes=["gfb"], dma="gfb")

        def rstd4(col, scale):
            P.op("act", lambda a: a.activation(sm4[:, col:col + 1], sm4[:, col:col + 1], AF.Ln, bias=epsT, scale=scale),
                 reads=[("sm4", col), "eps"], writes=[("sm4", col)])
            P.op("act", lambda a: a.activation(sm4[:, col:col + 1], sm4[:, col:col + 1], AF.Exp, scale=-0.5),
                 reads=[("sm4", col)], writes=[("sm4", col)])

        ntiles = opts.get('ntiles', NT if stage >= 8 else 2)
        junk5 = L.get([128, D], F32)
        pj = [L.get([128, D], F32) for _ in range(2)]

        def p4_front(T):
            s = T % 2
            hs = h2[s]
            P.op("sp", lambda q: q.dma_start(out=hs, in_=h2scr[T * 128:(T + 1) * 128, :]),
                 writes=[("h2", s)], dma=("h2", s))
            P.op("sp", lambda q: q.dma_start(out=pt_[s], in_=pin[T * 128:(T + 1) * 128, :]),
                 writes=[("pt", s)], dma=("pt", s))
            P.op("dve", lambda v: v.scalar_tensor_tensor(
                out=junk5, in0=hs, scalar=1.0, in1=hs, op0=ALU.mult, op1=ALU.mult, accum_out=sm4[:, 0:1]),
                reads=[("h2", s)], writes=["junk5", ("sm4", 0)])
            rstd4(0, 1.0 / D)
            P.op("act", lambda a: a.activation(hb, hs, AF.Copy, scale=sm4[:, 0:1]),
                 reads=[("h2", s), ("sm4", 0)], writes=["hb"])
            P.op("act", lambda a: a.copy(pb, pt_[s]), reads=[("pt", s)], writes=["pb"])

            def tr4(pe):
                last = None
                for c in range(8):
                    last = pe.transpose(psb(0)[:, c * 128:(c + 1) * 128], hb[:, c * 128:(c + 1) * 128], ident)
                return last
            P.op("pe", tr4, reads=["hb", "ident"], writes=[("ps", 0)])
            P.op("act", lambda a: a.copy(h3T, psb(0).rearrange("p (c t) -> p c t", c=8)), reads=[("ps", 0)], writes=["h3T"])

            def tr5(pe):
                last = None
                for c in range(2):
                    last = pe.transpose(psb(1)[:, c * 128:(c + 1) * 128], pb[:, c * 128:(c + 1) * 128], ident)
                return last
            P.op("pe", tr5, reads=["pb", "ident"], writes=[("ps", 1)])
            P.op("act", lambda a: a.copy(pT, psb(1)[:, 0:256].rearrange("p (c t) -> p c t", c=2)), reads=[("ps", 1)], writes=["pT"])
            for half in range(2):
                gb_ = (2 if T % 2 == 0 else 6) + half
                pb_ = 4 + half
                cs = slice(half * 512, (half + 1) * 512)

                def mg(pe, gb_=gb_, cs=cs):
                    last = None
                    for kc in range(8):
                        last = pe.matmul(psf(gb_), h3T[:, kc, :], wgT[:, kc, cs], start=(kc == 0), stop=(kc == 7))
                    return last
                P.op("pe", mg, reads=["h3T"] + [("wg", kc) for kc in range(8)], writes=[("ps", gb_)])

                def mp(pe, pb_=pb_, cs=cs):
                    last = None
                    for kc in range(2):
                        last = pe.matmul(psf(pb_), pT[:, kc, :], wpT[:, kc, cs], start=(kc == 0), stop=(kc == 1))
                    return last
                P.op("pe", mp, reads=["pT", ("wp", 0), ("wp", 1)], writes=[("ps", pb_)])
                P.op("act", lambda a, pb_=pb_, cs=cs: a.copy(pj[s][:, cs], psf(pb_)), reads=[("ps", pb_)], writes=[("pj", s, half)])

        def p4_back(T):
            s = T % 2
            hs = h2[s]
            for half in range(2):
                gb_ = (2 if T % 2 == 0 else 6) + half
                pb_ = 4 + half
                cs = slice(half * 512, (half + 1) * 512)
                P.op("dve", lambda v, gb_=gb_, cs=cs: v.tensor_tensor(gt[:, cs], psf(gb_), bgb[:, cs], ALU.add),
                     reads=[("ps", gb_), "bgb"], writes=[("gt", half)])
                P.op("act", lambda a, cs=cs: a.activation(gt[:, cs], gt[:, cs], AF.Exp, scale=-1.0),
                     reads=[("gt", half)], writes=[("gt", half)])
                P.op("act", lambda a, cs=cs: a.activation(gt[:, cs], gt[:, cs], AF.Ln, bias=oneT, scale=1.0),
                     reads=[("gt", half), "one"], writes=[("gt", half)])
                P.op("act", lambda a, cs=cs: a.activation(gt[:, cs], gt[:, cs], AF.Exp, scale=-1.0),
                     reads=[("gt", half)], writes=[("gt", half)])
                P.op("dve", lambda v, cs=cs: v.tensor_tensor(gt[:, cs], gt[:, cs], pj[s][:, cs], ALU.mult),
                     reads=[("gt", half), ("pj", s, half)], writes=[("gt", half)])
                P.op("dve", lambda v, cs=cs: v.tensor_tensor(hs[:, cs], hs[:, cs], gt[:, cs], ALU.add),
                     reads=[("gt", half), ("h2", s)], writes=[("h2", s)])
            P.op("dve", lambda v: v.scalar_tensor_tensor(
                out=junk4, in0=hs, scalar=1.0, in1=hs, op0=ALU.mult, op1=ALU.mult, accum_out=sm4[:, 1:2]),
                reads=[("h2", s)], writes=["junk4", ("sm4", 1)])
            rstd4(1, 1.0 / D)
            P.op("dve", lambda v: v.scalar_tensor_tensor(
                out=ot[s], in0=hs, scalar=sm4[:, 1:2], in1=gfb, op0=ALU.mult, op1=ALU.mult),
                reads=[("h2", s), ("sm4", 1), "gfb"], writes=[("ot", s)])
            P.op("pool", lambda g: g.dma_start(out=out[T * 128:(T + 1) * 128, :], in_=ot[s]),
                 reads=[("ot", s)], dma=("ot", s))

        p4_front(0)
        for T in range(ntiles):
            if T + 1 < ntiles:
                p4_front(T + 1)
            p4_back(T)

    dbg_out = {}
    P.barrier()
    for name, (ap, shape, dt) in dbg.items():
        dd = nc.dram_tensor("dbg_" + name, shape, dt, kind="ExternalOutput").ap()
        dbg_out[name] = dd
        P.op("sp", lambda q, dd=dd, ap=ap: q.dma_start(out=dd, in_=ap), dma=("dbg", name))
    P.barrier()
    P.finalize()

    import contextlib
    with contextlib.ExitStack() as es:
        esems = {e: es.enter_context(nc.semaphore("e_" + e)) for e in ("pe", "act", "dve", "pool")}
        esems["sp"] = None
        dsems = {}
        for i, k in enumerate(P.dma_count.keys()):
            dsems[k] = es.enter_context(nc.semaphore("d%d" % i))
        block = es.enter_context(nc.Block())

        @block.tensor
        def _(pe):
            P.emit("pe", pe, esems, dsems)

        @block.scalar
        def _(act):
            P.emit("act", act, esems, dsems)

        @block.vector
        def _(dve):
            P.emit("dve", dve, esems, dsems)

        @block.gpsimd
        def _(pool):
            P.emit("pool", pool, esems, dsems)

        @block.sync
        def _(sp):
            P.emit("sp", sp, esems, dsems)
    return nc, dbg_out


def make_in_maps(inputs):
    f = lambda a: np.ascontiguousarray(np.asarray(a, dtype=np.float32))
    x = f(inputs["x"])
    p = f(inputs["p"])[0]
    shared = {
        "g_attn": f(inputs["g_attn"]).reshape(1, D),
        "w_in": f(inputs["w_in"])[0],
        "g_out": np.concatenate([f(inputs["g_out_a"]).reshape(-1), f(inputs["g_out_b"]).reshape(-1)]).reshape(1, D),
        "w_out": f(inputs["w_out"])[0],
        "g_mlp": f(inputs["g_mlp"]).reshape(1, D),
        "w_up": f(inputs["w_up"])[0],
        "w_down": f(inputs["w_down"])[0],
        "g_ple": f(inputs["g_ple"]).reshape(1, D),
        "w_gate": f(inputs["w_ple_gate"])[0],
        "b_gate": f(inputs["b_ple_gate"]).reshape(1, D),
        "w_proj": f(inputs["w_ple_proj"])[0],
        "g_final": f(inputs["g_final"]).reshape(1, D),
    }
    maps = []
    for c in range(8):
        m = dict(shared)
        m["x"] = np.ascontiguousarray(x[c])
        m["p"] = np.ascontiguousarray(p[c])
        maps.append(m)
    return maps


def kernel(**inputs):
    nc, _ = build_nc()
    in_maps = make_in_maps(inputs)
    res = run_bass_kernel_spmd(nc, in_maps, core_ids=list(range(8)))
    return np.stack([np.asarray(r["out"], dtype=np.float32) for r in res.results], axis=0)
```

```python
import numpy as np
import concourse.bass as bass
import concourse.mybir as mybir
from concourse.bass_utils import run_bass_kernel_spmd

F32 = mybir.dt.float32
BF16 = mybir.dt.bfloat16
U8 = mybir.dt.uint8
ALU = mybir.AluOpType
AF = mybir.ActivationFunctionType
AX = mybir.AxisListType

S = 4096
D = 1024
NT = S // 128
DFF = 4096
PLE = 256
EPS = 1e-6
SLOPE_A = [2.0 ** (-(h + 0.5)) for h in range(8)]
SLOPE_B = [2.0 ** (-(h + 1.0)) for h in range(8)]
DILS = (1, 4, 16)
ENGS = ("pe", "act", "dve", "pool", "sp")


class Op:
    __slots__ = ("eng", "fn", "deps", "signal", "is_dma", "key", "val", "idx")


class Prog:
    def __init__(self):
        self.ops = {e: [] for e in ENGS}
        self.lastw = {}
        self.readers = {}
        self.dma_last = {}
        self.dma_count = {}

    def op(self, eng, fn, reads=(), writes=(), after=(), dma=None):
        o = Op()
        o.eng = eng
        o.fn = fn
        o.signal = False
        o.is_dma = dma is not None
        o.key = dma
        o.val = 0
        ps_reads = [b for b in reads if isinstance(b, tuple) and b[0] == "ps"]
        if ps_reads:
            reads = [b for b in reads if b not in ps_reads]
            writes = list(writes) + ps_reads
        cand = []
        for b in reads:
            w = self.lastw.get(b)
            if w is not None:
                cand.append(w)
        for b in writes:
            w = self.lastw.get(b)
            if w is not None:
                cand.append(w)
            cand.extend(self.readers.get(b, ()))
        cand.extend(after)
        if dma is not None:
            p = self.dma_last.get(dma)
            if p is not None:
                cand.append(p)
            self.dma_last[dma] = o
            self.dma_count[dma] = self.dma_count.get(dma, 0) + 1
            o.val = 16 * self.dma_count[dma]
            o.signal = True
        best = {}
        for d in cand:
            if d is o:
                continue
            if d.is_dma:
                k = ("dma", d.key)
                if k not in best or best[k].val < d.val:
                    best[k] = d
            else:
                if d.eng == "pe" and eng == "pe" and dma is None:
                    continue
                k = ("eng", d.eng)
                if k not in best or best[k].idx < d.idx:
                    best[k] = d
        o.deps = list(best.values())
        for d in o.deps:
            d.signal = True
        for b in reads:
            self.readers.setdefault(b, []).append(o)
        for b in writes:
            self.lastw[b] = o
            self.readers[b] = []
        o.idx = len(self.ops[eng])
        self.ops[eng].append(o)
        return o

    def barrier(self):
        lasts = []
        for e in ENGS:
            for o in reversed(self.ops[e]):
                if o.fn is not None and not o.is_dma:
                    lasts.append(o)
                    break
        lasts.extend(self.dma_last.values())
        for e in ENGS:
            self.op(e, None, after=lasts)
        self.lastw = {}
        self.readers = {}

    def finalize(self):
        for e in ENGS:
            n = 0
            for o in self.ops[e]:
                if o.is_dma:
                    continue
                if o.signal and o.fn is not None:
                    n += 1
                    o.val = n

    def emit(self, e, eng, esems, dsems):
        known = {}
        for o in self.ops[e]:
            for d in o.deps:
                if d.is_dma:
                    k = ("dma", d.key)
                    sem = dsems[d.key]
                else:
                    k = ("eng", d.eng)
                    sem = esems[d.eng]
                if known.get(k, 0) >= d.val:
                    continue
                eng.wait_ge(sem, d.val)
                known[k] = d.val
            if o.fn is None:
                continue
            last = o.fn(eng)
            if o.signal:
                if o.is_dma:
                    last.then_inc(dsems[o.key], 16)
                else:
                    last.then_inc(esems[e], 1)


class Arena:
    def __init__(self, nc, nbytes):
        self.t = nc.alloc_sbuf_tensor("arena", [128, nbytes], U8)
        self.n = nbytes

    def view(self, off, shape, dt):
        sz = 4 if dt == F32 else 2
        n = int(np.prod(shape[1:]))
        assert off % 4 == 0 and off + n * sz <= self.n, (off, shape, self.n)
        v = self.t[:, off:off + n * sz].bitcast(dt)
        if len(shape) > 2:
            names = " ".join("d%d" % i for i in range(1, len(shape)))
            kw = {"d%d" % i: shape[i] for i in range(1, len(shape))}
            v = v.rearrange("p (%s) -> p %s" % (names, names), **kw)
        return v


class Lay:
    def __init__(self, arena, start, limit):
        self.a = arena
        self.o = start
        self.limit = limit

    def get(self, shape, dt):
        sz = 4 if dt == F32 else 2
        n = int(np.prod(shape[1:])) * sz
        n = (n + 31) // 32 * 32
        v = self.a.view(self.o, shape, dt)
        self.o += n
        assert self.o <= self.limit, (self.o, self.limit)
        return v


def build_nc(stage=99, opts=None):
    opts = opts or {}
    nc = bass.Bass("TRN2", target_bir_lowering=False)
    P = Prog()

    def dram_in(name, shape):
        return nc.dram_tensor(name, shape, F32, kind="ExternalInput").ap()

    x = dram_in("x", [S, D])
    pin = dram_in("p", [S, PLE])
    g_attn = dram_in("g_attn", [1, D])
    w_in = dram_in("w_in", [D, 3 * D])
    g_out = dram_in("g_out", [1, D])
    w_out = dram_in("w_out", [D, D])
    g_mlp = dram_in("g_mlp", [1, D])
    w_up = dram_in("w_up", [D, DFF])
    w_down = dram_in("w_down", [DFF, D])
    g_ple = dram_in("g_ple", [1, D])
    w_gate = dram_in("w_gate", [D, D])
    b_gate = dram_in("b_gate", [1, D])
    w_proj = dram_in("w_proj", [PLE, D])
    g_final = dram_in("g_final", [1, D])
    out = nc.dram_tensor("out", [S, D], F32, kind="ExternalOutput").ap()
    skind = "ExternalOutput" if stage < 99 else "Internal"
    uscr = nc.dram_tensor("uscr", [4, S, 520], F32, kind=skind).ap()
    h2scr = nc.dram_tensor("h2scr", [S, D], F32, kind=skind).ap()

    ARENA = 212000
    ar = Arena(nc, ARENA)
    psall = nc.alloc_psum_tensor("psall", [128, 4096], F32)

    def psf(i):
        return psall[:, i * 512:(i + 1) * 512]

    def psb(i):
        return psall[:, i * 512:(i + 1) * 512].bitcast(BF16)

    L = Lay(ar, 0, 12288)
    ident = L.get([128, 128], BF16)
    epsT = L.get([128, 1], F32)
    oneT = L.get([128, 1], F32)
    gA = L.get([128, 8], F32)
    gM = L.get([128, 8], F32)
    gP = L.get([128, 8], F32)
    gO = L.get([128, 8], F32)
    CONST_KEEP = L.o
    D0 = L.get([128, 128], F32)
    Dpos = L.get([128, 128], F32)
    Dneg = L.get([128, 128], F32)
    D128 = L.get([128, 128], F32)
    Mcur = L.get([128, 128], F32)
    Mprev = L.get([128, 128], F32)
    facv = L.get([128, 2], F32)
    fac = L.get([128, 8, 2], F32)
    vgm = L.get([128, 32, 16], F32)
    negm = L.get([128, 32, 16], F32)
    CONST_END = L.o

    def cop(eng, fn, reads=(), writes=()):
        return P.op(eng, fn, reads, writes)

    cop("pool", lambda g: g.iota(D0, [[1, 128]], base=0, channel_multiplier=-1,
                                 allow_small_or_imprecise_dtypes=True), writes=["D0"])
    cop("pool", lambda g: g.iota(facv, [[128, 2]], base=-255, channel_multiplier=1,
                                 allow_small_or_imprecise_dtypes=True), writes=["facv"])
    cop("pool", lambda g: g.iota(vgm, [[128, 32], [-256, 16]], base=-255, channel_multiplier=1,
                                 allow_small_or_imprecise_dtypes=True), writes=["vgm0"])
    cop("pool", lambda g: g.iota(negm, [[1, 16], [0, 2], [-1, 16]], base=0, channel_multiplier=0,
                                 allow_small_or_imprecise_dtypes=True), writes=["negm0"])
    cop("dve", lambda v: v.memset(epsT, EPS), writes=["eps"])
    cop("dve", lambda v: v.memset(oneT, 1.0), writes=["one"])
    cop("dve", lambda v: v.tensor_scalar(ident, D0, 0.0, None, ALU.is_equal), reads=["D0"], writes=["ident"])
    cop("dve", lambda v: v.tensor_scalar(Dpos, D0, 0.0, None, ALU.max), reads=["D0"], writes=["Dpos"])
    cop("dve", lambda v: v.tensor_scalar(Dneg, D0, 0.0, 128.0, ALU.min, ALU.add), reads=["D0"], writes=["Dneg"])
    cop("dve", lambda v: v.tensor_scalar(D128, D0, 128.0, None, ALU.add), reads=["D0"], writes=["D128"])
    cop("dve", lambda v: v.tensor_scalar(Mcur, D0, 0.0, None, ALU.is_ge), reads=["D0"], writes=["Mcur"])
    cop("dve", lambda v: v.tensor_scalar(Mprev, D0, 0.0, None, ALU.is_le), reads=["D0"], writes=["Mprev"])
    cop("dve", lambda v: v.tensor_scalar(vgm, vgm, 0.0, None, ALU.max), reads=["vgm0"], writes=["vgm"])
    cop("dve", lambda v: v.tensor_scalar(negm, negm, 0.5, -1e30, ALU.is_lt, ALU.mult), reads=["negm0"], writes=["negm"])
    for h in range(8):
        cop("act", lambda a, h=h: a.activation(fac[:, h, :], facv, AF.Exp, scale=SLOPE_B[h]),
            reads=["facv"], writes=[("fac", h)])

    def gload(dst, src, key):
        P.op("sp", lambda q: q.dma_start(out=dst, in_=src.rearrange("o (c p) -> p (o c)", p=128),
                                         allow_slow_non_contiguous=True),
             writes=[key], dma=("g", key))

    gload(gA, g_attn, "gA")
    gload(gM, g_mlp, "gM")
    gload(gP, g_ple, "gP")
    gload(gO, g_out, "gO")

    dbg = {}

    L = Lay(ar, CONST_END, ARENA)
    hnT = L.get([128, 8, S], BF16)
    A0 = L.o
    xt = [L.get([128, D], F32) for _ in range(2)]
    hnb = [L.get([128, D], BF16) for _ in range(2)]
    ssq1 = L.get([128, NT], F32)
    rs1 = L.get([128, NT], F32)
    junk = L.get([128, D], F32)

    for T in range(NT):
        sl = T % 2
        P.op("sp", lambda q, T=T, sl=sl: q.dma_start(out=xt[sl], in_=x[T * 128:(T + 1) * 128, :]),
             writes=[("xt", sl)], dma=("xt", sl))
        P.op("dve", lambda v, T=T, sl=sl: v.scalar_tensor_tensor(
            out=junk, in0=xt[sl], scalar=1.0, in1=xt[sl], op0=ALU.mult, op1=ALU.mult,
            accum_out=ssq1[:, T:T + 1]), reads=[("xt", sl)], writes=["junk", ("ssq1", T)])
        P.op("act", lambda a, T=T: a.activation(rs1[:, T:T + 1], ssq1[:, T:T + 1], AF.Ln, bias=epsT, scale=1.0 / D),
             reads=[("ssq1", T), "eps"], writes=[("rs1", T)])
        P.op("act", lambda a, T=T: a.activation(rs1[:, T:T + 1], rs1[:, T:T + 1], AF.Exp, scale=-0.5),
             reads=[("rs1", T)], writes=[("rs1", T)])
        P.op("dve", lambda v, T=T, sl=sl: v.tensor_scalar(hnb[sl], xt[sl], rs1[:, T:T + 1], None, ALU.mult),
             reads=[("xt", sl), ("rs1", T)], writes=[("hnb", sl)])
        bank = T % 2

        def tr(pe, sl=sl, bank=bank):
            last = None
            for c in range(8):
                last = pe.transpose(psb(bank)[:, c * 128:(c + 1) * 128], hnb[sl][:, c * 128:(c + 1) * 128], ident)
            return last
        P.op("pe", tr, reads=[("hnb", sl), "ident"], writes=[("ps", bank)])
        src = psb(bank).rearrange("p (c t) -> p c t", c=8)
        dst = hnT[:, :, T * 128:(T + 1) * 128]
        if T % 2 == 0:
            P.op("act", lambda a, src=src, dst=dst: a.copy(dst, src), reads=[("ps", bank)], writes=[("hnT", T // 4)])
        else:
            P.op("dve", lambda v, src=src, dst=dst: v.tensor_copy(dst, src), reads=[("ps", bank)], writes=[("hnT", T // 4)])

    if stage == 1:
        dbg["hnT"] = (hnT, [128, 8, S], BF16)

    L = Lay(ar, A0, ARENA)
    wst = [L.get([128, 8, 384], F32) for _ in range(2)]
    wpr = [L.get([128, 8, 384], BF16) for _ in range(2)]
    QT = L.get([128, S], BF16)
    KT = L.get([128, S], BF16)
    VT = L.get([128, S], BF16)
    Vaug = L.get([128, 3, 32, 2, 65], BF16)
    PT = [L.get([128, 512], BF16) for _ in range(3)]
    WA = L.get([128, 4, 3, 512], BF16)
    WD = L.get([128, 8, 384], BF16)
    Uev = [L.get([128, 130], F32) for _ in range(4)]
    etmp = [L.get([128, 128], F32) for _ in range(2)]
    ksum = L.get([128, 16], F32)
    kmT = L.get([128, 16], BF16)
    gm = L.get([128, 2, 32, 16], F32)
    t8 = L.get([128, 2, 32, 8], F32)
    cT = L.get([128, 2, 32, 16], F32)
    accB = [L.get([128, 2, 2, 65], F32) for _ in range(2)]

    if stage >= 2:
        P.barrier()
        P.op("pool", lambda g: g.memset(Vaug[:, :, :, :, 64:65], 1.0), writes=["vones"])
        k = 0
        for pr in range(4):
            for bi, dil in enumerate(DILS):
                for hh in range(2):
                    sl_ = SLOPE_A[2 * pr + hh]
                    for which, (dsrc, msk, dkey, mkey) in enumerate(((Dpos, Mcur, "Dpos", "Mcur"), (Dneg, Mprev, "Dneg", "Mprev"))):
                        e = k % 2
                        k += 1
                        col = hh * 256 + which * 128
                        P.op("act", lambda a, e=e, dsrc=dsrc, sc=-sl_ * dil: a.activation(etmp[e], dsrc, AF.Exp, scale=sc),
                             reads=[dkey], writes=[("etmp", e)])
                        P.op("dve", lambda v, e=e, msk=msk, pr=pr, bi=bi, col=col: v.tensor_tensor(
                            WA[:, pr, bi, col:col + 128], etmp[e], msk, ALU.mult),
                            reads=[("etmp", e), mkey], writes=["WA"])
        for h in range(8):
            e = k % 2
            k += 1
            P.op("act", lambda a, e=e, h=h: a.activation(etmp[e], Dpos, AF.Exp, scale=-SLOPE_B[h]),
                 reads=["Dpos"], writes=[("etmp", e)])
            P.op("dve", lambda v, e=e, h=h: v.tensor_tensor(WD[:, h, 0:128], etmp[e], Mcur, ALU.mult),
                 reads=[("etmp", e), "Mcur"], writes=["WD"])
            P.op("dve", lambda v, e=e, h=h: v.tensor_tensor(WD[:, h, 256:384], etmp[e], Mcur, ALU.mult),
                 reads=[("etmp", e), "Mcur"], writes=["WD"])
            P.op("act", lambda a, h=h: a.activation(WD[:, h, 128:256], D128, AF.Exp, scale=-SLOPE_B[h]),
                 reads=["D128"], writes=["WD"])

    rot = {"proj": 0, "S": 0, "O": 0, "PT": 0, "U": 0, "ev": 0}

    def nxt(name, n):
        v = rot[name]
        rot[name] = (v + 1) % n
        return v

    def load_wpair(pi):
        slot = pi % 2
        grp, pr = divmod(pi, 4)
        base = grp * 1536 + pr * 128
        for w in range(3):
            c0 = base + w * 512
            P.op("sp", lambda q, slot=slot, w=w, c0=c0: q.dma_start(
                out=wst[slot][:, :, w * 128:(w + 1) * 128],
                in_=w_in[:, c0:c0 + 128].rearrange("(c p) n -> p c n", p=128)),
                writes=[("wst", slot, w)], dma=("wst", slot, w))
            eng = ("dve", "pool", "dve")[w]
            P.op(eng, lambda v, slot=slot, w=w: v.tensor_tensor(
                wpr[slot][:, :, w * 128:(w + 1) * 128], wst[slot][:, :, w * 128:(w + 1) * 128],
                gA.unsqueeze(2).to_broadcast([128, 8, 128]), ALU.mult),
                reads=[("wst", slot, w), "gA"], writes=[("wpr", slot, w)])

    def project(pi, is_b):
        slot = pi % 2
        for w, dst, key in ((0, QT, "QT"), (1, KT, "KT"), (2, VT, "VT")):
            for tc in range(8):
                bank = nxt("proj", 2)

                def mm(pe, slot=slot, w=w, tc=tc, bank=bank):
                    last = None
                    for kc in range(8):
                        last = pe.matmul(psf(bank), wpr[slot][:, kc, w * 128:(w + 1) * 128],
                                         hnT[:, kc, tc * 512:(tc + 1) * 512], start=(kc == 0), stop=(kc == 7))
                    return last
                P.op("pe", mm, reads=[("wpr", slot, w), ("hnT", tc)], writes=[("ps", bank)])
                d = dst[:, tc * 512:(tc + 1) * 512]
                if tc % 2 == 0:
                    P.op("act", lambda a, d=d, bank=bank: a.copy(d, psf(bank)), reads=[("ps", bank)], writes=[(key, tc)])
                else:
                    P.op("dve", lambda v, d=d, bank=bank: v.tensor_copy(d, psf(bank)), reads=[("ps", bank)], writes=[(key, tc)])
                if is_b and w == 1:
                    P.op("dve", lambda v, tc=tc, bank=bank: v.tensor_reduce(
                        ksum[:, tc * 2:(tc + 1) * 2], psf(bank).rearrange("p (a b) -> p a b", a=2), AX.X, ALU.add),
                        reads=[("ps", bank)], writes=[("ksum", tc)])

    def tok_ap(t, dil, r, n):
        st = r + 128 * dil * n
        return t[:, st:st + 127 * dil + 1:dil] if dil > 1 else t[:, st:st + 128]

    def blocks(dil):
        nb = 32 // dil
        return [(r, n) for r in range(dil) for n in range(nb)]

    def build_vaug(ords):
        for oi, dil in ords:
            bl = blocks(dil)
            for g0 in range(0, 32, 8):
                bank = nxt("ev", 2)

                def tr(pe, dil=dil, g0=g0, bank=bank, bl=bl):
                    last = None
                    for j in range(8):
                        r, n = bl[g0 + j]
                        last = pe.transpose(psb(bank)[:, j * 128:(j + 1) * 128], tok_ap(VT, dil, r, n), ident)
                    return last
                P.op("pe", tr, reads=[("VT", i) for i in range(8)] + ["ident"], writes=[("ps", bank)])
                src = psb(bank).rearrange("p (j h d) -> p j h d", j=8, h=2)
                dst = Vaug[:, oi, g0:g0 + 8, :, 0:64]
                if (g0 // 8) % 2 == 0:
                    P.op("dve", lambda v, src=src, dst=dst: v.tensor_copy(dst, src),
                         reads=[("ps", bank), "vones"], writes=[("Vaug", oi)])
                else:
                    P.op("act", lambda a, src=src, dst=dst: a.copy(dst, src),
                         reads=[("ps", bank), "vones"], writes=[("Vaug", oi)])

    allkeys = lambda name: [(name, i) for i in range(8)]

    def pipeline(iters, depth=2):
        n = len(iters)
        for i in range(n + depth):
            if i < n:
                iters[i][0]()
            if i - depth >= 0:
                iters[i - depth][1]()

    def attn_dilated(pr):
        iters = []
        for bi, dil in enumerate(DILS):
            bl = blocks(dil)
            bidx = {b: i for i, b in enumerate(bl)}
            for (r, n) in bl:
                ss = nxt("S", 2)
                sb0 = 2 + 2 * ss
                ob = 6 + nxt("O", 2)
                pt = nxt("PT", 3)
                us = nxt("U", 4)
                ncol = 256 if n > 0 else 128

                def stA(bi=bi, dil=dil, r=r, n=n, sb0=sb0, pt=pt, ncol=ncol):
                    def sc(pe):
                        last = None
                        for hh in range(2):
                            rows = slice(hh * 64, hh * 64 + 64)
                            qa = tok_ap(QT, dil, r, n)[rows, :]
                            last = pe.matmul(psf(sb0 + hh)[:, 0:128], tok_ap(KT, dil, r, n)[rows, :], qa,
                                             start=True, stop=True)
                            if n > 0:
                                last = pe.matmul(psf(sb0 + hh)[:, 128:256],
                                                 tok_ap(KT, dil, r, n - 1)[rows, :], qa, start=True, stop=True)
                        return last
                    P.op("pe", sc, reads=allkeys("QT") + allkeys("KT"), writes=[("ps", sb0), ("ps", sb0 + 1)])
                    src = psall[:, sb0 * 512:(sb0 + 2) * 512].rearrange("p (h c) -> p h c", h=2)[:, :, 0:ncol]
                    ptv = PT[pt].rearrange("p (h c) -> p h c", h=2)[:, :, 0:ncol]
                    wav = WA[:, pr, bi, :].rearrange("p (h c) -> p h c", h=2)[:, :, 0:ncol]
                    P.op("act", lambda a: a.activation(ptv, src, AF.Exp, scale=0.125),
                         reads=[("ps", sb0), ("ps", sb0 + 1)], writes=[("PT", pt)])
                    P.op("dve", lambda v: v.tensor_tensor(ptv, ptv, wav, ALU.mult),
                         reads=[("PT", pt), "WA"], writes=[("PT", pt)])

                def stB(bi=bi, dil=dil, r=r, n=n, ob=ob, pt=pt, us=us, bidx=bidx):
                    def pv(pe):
                        last = None
                        for hh in range(2):
                            o = psf(ob)[:, hh * 65:(hh + 1) * 65]
                            last = pe.matmul(o, PT[pt][:, hh * 256:hh * 256 + 128], Vaug[:, bi, bidx[(r, n)], hh, :],
                                             start=True, stop=(n == 0))
                            if n > 0:
                                last = pe.matmul(o, PT[pt][:, hh * 256 + 128:hh * 256 + 256],
                                                 Vaug[:, bi, bidx[(r, n - 1)], hh, :], start=False, stop=True)
                        return last
                    P.op("pe", pv, reads=[("PT", pt), ("Vaug", bi)], writes=[("ps", ob)])
                    P.op("dve", lambda v: v.tensor_copy(Uev[us], psf(ob)[:, 0:130]),
                         reads=[("ps", ob)], writes=[("Uev", us)])
                    st = r + 128 * dil * n
                    dst = uscr[bi, st:st + 127 * dil + 1:dil, pr * 130:(pr + 1) * 130] if dil > 1 else \
                        uscr[bi, st:st + 128, pr * 130:(pr + 1) * 130]
                    P.op("pool", lambda g: g.dma_start(out=dst, in_=Uev[us]),
                         reads=[("Uev", us)], dma=("Uev", us))
                iters.append((stA, stB))
        pipeline(iters)

    def attn_moba(pr):
        for hh in range(2):
            h = 2 * pr + hh
            for j in range(2):
                eng = "pool" if j == 0 else "dve"
                P.op(eng, lambda v, hh=hh, h=h, j=j: v.tensor_scalar(
                    Vaug[:, 1, j:32:2, hh, :], Vaug[:, 0, j:32:2, hh, :], fac[:, h, j:j + 1], None, ALU.mult),
                    reads=[("Vaug", 0), ("fac", h), "vones"], writes=[("Vaug", 1)])
        P.op("dve", lambda v: v.tensor_scalar(kmT, ksum, 1.0 / 256.0, None, ALU.mult),
             reads=[("ksum", i) for i in range(8)], writes=["kmT"])
        for hh in range(2):
            h = 2 * pr + hh
            rows = slice(hh * 64, hh * 64 + 64)
            gb = hh

            def gmm(pe, rows=rows, gb=gb):
                last = None
                for qt in range(32):
                    last = pe.matmul(psf(gb)[:, qt * 16:(qt + 1) * 16], QT[rows, qt * 128:(qt + 1) * 128], kmT[rows, :],
                                     start=True, stop=True)
                return last
            P.op("pe", gmm, reads=allkeys("QT") + ["kmT"], writes=[("ps", gb)])
            P.op("dve", lambda v, hh=hh, gb=gb: v.tensor_tensor(
                gm[:, hh], psf(gb).rearrange("p (a b) -> p a b", a=32), negm, ALU.add),
                reads=[("ps", gb), "negm"], writes=[("gm", hh)])

            def mx(v, hh=hh):
                last = None
                for qt in range(32):
                    last = v.max(t8[:, hh, qt, :], gm[:, hh, qt, :])
                return last
            P.op("dve", mx, reads=[("gm", hh)], writes=[("t8", hh)])
            P.op("act", lambda a, hh=hh, h=h: a.activation(cT[:, hh], vgm, AF.Exp, scale=-SLOPE_B[h]),
                 reads=["vgm"], writes=[("cT", hh)])
            P.op("dve", lambda v, hh=hh: v.tensor_tensor(
                gm[:, hh], gm[:, hh], t8[:, hh, :, 2:3].to_broadcast([128, 32, 16]), ALU.is_ge),
                reads=[("gm", hh), ("t8", hh)], writes=[("gm", hh)])
            P.op("dve", lambda v, hh=hh: v.tensor_tensor(cT[:, hh], cT[:, hh], gm[:, hh], ALU.mult),
                 reads=[("gm", hh), ("cT", hh)], writes=[("cT", hh)])
        iters = []
        nbq = opts.get('moba_nb', 16)
        for b in range(nbq):
            ab = b % 2
            for hh in range(2):
                h = 2 * pr + hh
                rows = slice(hh * 64, hh * 64 + 64)
                sb = 2 + nxt("S", 4)
                ob = 6 + nxt("O", 2)
                pt = nxt("PT", 3)

                def dA(b=b, rows=rows, sb=sb, pt=pt, h=h):
                    def scd(pe):
                        pe.matmul(psf(sb)[:, 0:256], KT[rows, b * 256:b * 256 + 128], QT[rows, b * 256:(b + 1) * 256],
                                  start=True, stop=True)
                        return pe.matmul(psf(sb)[:, 256:384], KT[rows, b * 256 + 128:(b + 1) * 256],
                                         QT[rows, b * 256 + 128:(b + 1) * 256], start=True, stop=True)
                    P.op("pe", scd, reads=allkeys("QT") + allkeys("KT"), writes=[("ps", sb)])
                    P.op("act", lambda a: a.activation(PT[pt][:, 0:384], psf(sb)[:, 0:384], AF.Exp, scale=0.125),
                         reads=[("ps", sb)], writes=[("PT", pt)])
                    P.op("dve", lambda v: v.tensor_tensor(PT[pt][:, 0:384], PT[pt][:, 0:384], WD[:, h, :], ALU.mult),
                         reads=[("PT", pt), "WD"], writes=[("PT", pt)])

                def dB(b=b, hh=hh, ob=ob, pt=pt, ab=ab):
                    def pvd(pe):
                        pe.matmul(psf(ob)[:, 0:65], PT[pt][:, 0:128], Vaug[:, 0, 2 * b, hh, :], start=True, stop=True)
                        pe.matmul(psf(ob)[:, 65:130], PT[pt][:, 128:256], Vaug[:, 0, 2 * b, hh, :], start=True, stop=False)
                        return pe.matmul(psf(ob)[:, 65:130], PT[pt][:, 256:384], Vaug[:, 0, 2 * b + 1, hh, :], start=False, stop=True)
                    P.op("pe", pvd, reads=[("PT", pt), ("Vaug", 0)], writes=[("ps", ob)])
                    P.op("dve", lambda v: v.tensor_copy(
                        accB[ab][:, :, hh, :], psf(ob)[:, 0:130].rearrange("p (j d) -> p j d", j=2)),
                        reads=[("ps", ob)], writes=[("acc", ab, hh)])
                iters.append((dA, dB))
                for n in range(b):
                    sb = 2 + nxt("S", 4)
                    ob = 6 + nxt("O", 2)
                    pt = nxt("PT", 3)

                    def oA(b=b, n=n, rows=rows, sb=sb, pt=pt):
                        def sco(pe):
                            q = QT[rows, b * 256:(b + 1) * 256]
                            pe.matmul(psf(sb)[:, 0:256], KT[rows, n * 256:n * 256 + 128], q, start=True, stop=True)
                            return pe.matmul(psf(sb)[:, 256:512], KT[rows, n * 256 + 128:(n + 1) * 256], q, start=True, stop=True)
                        P.op("pe", sco, reads=allkeys("QT") + allkeys("KT"), writes=[("ps", sb)])
                        P.op("act", lambda a: a.activation(PT[pt], psf(sb), AF.Exp, scale=0.125),
                             reads=[("ps", sb)], writes=[("PT", pt)])

                    def oB(b=b, n=n, hh=hh, ob=ob, pt=pt, ab=ab):
                        def pvo(pe):
                            last = None
                            for j in range(2):
                                o = psf(ob)[:, j * 65:(j + 1) * 65]
                                pe.matmul(o, PT[pt][:, j * 128:(j + 1) * 128], Vaug[:, 1, 2 * n, hh, :], start=True, stop=False)
                                last = pe.matmul(o, PT[pt][:, 256 + j * 128:256 + (j + 1) * 128], Vaug[:, 1, 2 * n + 1, hh, :],
                                                 start=False, stop=True)
                            return last
                        P.op("pe", pvo, reads=[("PT", pt), ("Vaug", 1)], writes=[("ps", ob)])

                        def accf(v):
                            last = None
                            for j in range(2):
                                last = v.scalar_tensor_tensor(
                                    out=accB[ab][:, j, hh, :], in0=psf(ob)[:, j * 65:(j + 1) * 65],
                                    scalar=cT[:, hh, 2 * b + j, n:n + 1], in1=accB[ab][:, j, hh, :],
                                    op0=ALU.mult, op1=ALU.add)
                            return last
                        P.op("dve", accf, reads=[("ps", ob), ("cT", hh), ("acc", ab, hh)], writes=[("acc", ab, hh)])
                    iters.append((oA, oB))

            def stA(): pass

            def stB(b=b, ab=ab):
                dst = uscr[3, b * 256:(b + 1) * 256, pr * 130:(pr + 1) * 130]
                P.op("pool", lambda g: g.dma_start(
                    out=dst.rearrange("(j p) c -> p j c", p=128), in_=accB[ab].rearrange("p j h d -> p j (h d)")),
                    reads=[("acc", ab, 0), ("acc", ab, 1)], dma=("acc", ab))
            iters.append((stA, stB))
        pipeline(iters)

    if stage >= 2:
        plist = opts.get('pairs', {2: [0], 3: [0, 1, 2, 3]}.get(stage, list(range(8))))
        load_wpair(plist[0])
        for ii, pi in enumerate(plist):
            if ii + 1 < len(plist):
                load_wpair(plist[ii + 1])
            is_b = pi >= 4
            project(pi, is_b)
            if stage == 2:
                dbg["QT"] = (QT, [128, S], BF16)
                dbg["KT"] = (KT, [128, S], BF16)
                dbg["VT"] = (VT, [128, S], BF16)
            if not is_b:
                build_vaug([(0, 1), (1, 4), (2, 16)])
                attn_dilated(pi)
            else:
                build_vaug([(0, 1)])
                attn_moba(pi - 4)

    if stage >= 5 and opts.get('p3', True):
        P.barrier()
        L = Lay(ar, CONST_KEEP, ARENA)
        woT = L.get([128, 8, D], BF16)
        wuT = L.get([128, 8, DFF], BF16)
        wdT = L.get([128, 32, D], BF16)
        stg = [L.get([128, 512], F32) for _ in range(2)]
        Ut = L.get([128, 4, 520], F32)
        xh = [L.get([128, D], F32) for _ in range(4)]
        ynb = L.get([128, D], BF16)
        ynT = L.get([128, 8, 128], BF16)
        hn2T = L.get([128, 8, 256], BF16)
        actT = L.get([128, 32, 256], BF16)
        rtmp = [L.get([128, 512], F32) for _ in range(2)]
        stg = stg + rtmp
        sm = L.get([128, 16], F32)
        rec = L.get([128, 16], F32)
        cv = {"i": 0}

        def conv_w(dst3, src2, nk, ncols, gt, key, colmajor=False):
            step = 512
            order = [(kc, c0) for c0 in range(0, ncols, step) for kc in range(nk)] if colmajor else \
                [(kc, c0) for kc in range(nk) for c0 in range(0, ncols, step)]
            for kc, c0 in order:
                w = min(step, ncols - c0)
                s = cv["i"] % 4
                e = ("dve", "act")[cv["i"] % 2]
                cv["i"] += 1
                wkey = (key, c0 // step) if colmajor else (key, kc)
                P.op("sp", lambda q, s=s, kc=kc, c0=c0, w=w: q.dma_start(
                    out=stg[s][:, 0:w], in_=src2[kc * 128:(kc + 1) * 128, c0:c0 + w]),
                    writes=[("stg", s)], dma=("stg", s))
                d = dst3[:, kc, c0:c0 + w]
                if gt is None:
                    if e == "act":
                        P.op(e, lambda a, s=s, d=d, w=w: a.copy(d, stg[s][:, 0:w]), reads=[("stg", s)], writes=[wkey])
                    else:
                        P.op(e, lambda v, s=s, d=d, w=w: v.tensor_copy(d, stg[s][:, 0:w]), reads=[("stg", s)], writes=[wkey])
                else:
                    gk, gkey = gt
                    sc = gk[:, kc:kc + 1]
                    if e == "act":
                        P.op(e, lambda a, s=s, d=d, w=w, sc=sc: a.activation(d, stg[s][:, 0:w], AF.Copy, scale=sc),
                             reads=[("stg", s), gkey], writes=[wkey])
                    else:
                        P.op(e, lambda v, s=s, d=d, w=w, sc=sc: v.tensor_scalar(d, stg[s][:, 0:w], sc, None, ALU.mult),
                             reads=[("stg", s), gkey], writes=[wkey])

        conv_w(woT, w_out, 8, D, (gO, "gO"), "wo")
        conv_w(wuT, w_up, 8, DFF, (gM, "gM"), "wu", colmajor=True)
        conv_w(wdT, w_down, 32, D, None, "wd")

        def rstd_ops(col, scale):
            P.op("act", lambda a: a.activation(sm[:, col:col + 1], sm[:, col:col + 1], AF.Ln, bias=epsT, scale=scale),
                 reads=[("sm", col), "eps"], writes=[("sm", col)])
            P.op("act", lambda a: a.activation(sm[:, col:col + 1], sm[:, col:col + 1], AF.Exp, scale=-0.5),
                 reads=[("sm", col)], writes=[("sm", col)])

        def transposes8(src_bf, dst3, dkey, bank):
            def tr(pe):
                last = None
                for c in range(8):
                    last = pe.transpose(psb(bank)[:, c * 128:(c + 1) * 128], src_bf[:, c * 128:(c + 1) * 128], ident)
                return last
            return tr

        ngroups = opts.get('ngroups', 16 if stage >= 6 else 1)
        ynbA = [ynb[:, 0:D], L.get([128, D], BF16)]
        ynTs = [ynT, L.get([128, 8, 128], BF16)]
        hnb2 = ynbA
        TRB = (0, 7)

        def front_s0(G):
            for t in range(2):
                T = G * 2 + t
                xi = (G % 2) * 2 + t
                xs = xh[xi]
                P.op("sp", lambda q, T=T: q.dma_start(
                    out=Ut, in_=uscr[:, T * 128:(T + 1) * 128, :].rearrange("s p c -> p s c")),
                    writes=["Ut"], dma="Ut")
                P.op("sp", lambda q, T=T, xs=xs: q.dma_start(out=xs, in_=x[T * 128:(T + 1) * 128, :]),
                     writes=[("xh", xi)], dma=("xh", xi))
                P.op("dve", lambda v: v.tensor_tensor(Ut[:, 0], Ut[:, 0], Ut[:, 1], ALU.add), reads=["Ut"], writes=["Ut"])
                P.op("dve", lambda v: v.tensor_tensor(Ut[:, 0], Ut[:, 0], Ut[:, 2], ALU.add), reads=["Ut"], writes=["Ut"])
                jk = Ut[:, 1, 0:512]
                for gi, us in ((0, 0), (1, 3)):
                    Uv = Ut[:, us].rearrange("p (h d) -> p h d", h=8)
                    Ov = Uv[:, :, 0:64]
                    col = t * 2 + gi
                    P.op("dve", lambda v, gi=gi, Uv=Uv: v.reciprocal(rec[:, gi * 8:(gi + 1) * 8], Uv[:, :, 64]),
                         reads=["Ut"], writes=[("rec", gi)])
                    P.op("dve", lambda v, gi=gi, Ov=Ov: v.tensor_tensor(
                        Ov, Ov, rec[:, gi * 8:(gi + 1) * 8].unsqueeze(2).to_broadcast([128, 8, 64]), ALU.mult),
                        reads=["Ut", ("rec", gi)], writes=["Ut"])
                    P.op("dve", lambda v, col=col, Ov=Ov, jk=jk: v.scalar_tensor_tensor(
                        out=jk.rearrange("p (h d) -> p h d", h=8), in0=Ov, scalar=1.0,
                        in1=Ov, op0=ALU.mult, op1=ALU.mult, accum_out=sm[:, col:col + 1]),
                        reads=["Ut"], writes=["Ut", ("sm", col)])
                    rstd_ops(col, 1.0 / 512)
                    P.op("dve", lambda v, gi=gi, col=col, Ov=Ov, t=t: v.tensor_scalar(
                        ynbA[t][:, gi * 512:(gi + 1) * 512].rearrange("p (h d) -> p h d", h=8), Ov, sm[:, col:col + 1], None, ALU.mult),
                        reads=["Ut", ("sm", col)], writes=[("ynbA", t, gi)])

        def front_s1(G):
            for t in range(2):
                P.op("pe", transposes8(ynbA[t], None, None, TRB[t]), reads=[("ynbA", t, 0), ("ynbA", t, 1), "ident"], writes=[("ps", TRB[t])])
                P.op("act", lambda a, t=t: a.copy(ynTs[t], psb(TRB[t]).rearrange("p (c t) -> p c t", c=8)),
                     reads=[("ps", TRB[t])], writes=[("ynT", t)])
            for t in range(2):
                xi = (G % 2) * 2 + t
                xs = xh[xi]
                for half in range(2):
                    bank = 1 + half

                    def mo(pe, half=half, bank=bank, t=t):
                        last = None
                        for kc in range(8):
                            last = pe.matmul(psf(bank), ynTs[t][:, kc, :], woT[:, kc, half * 512:(half + 1) * 512],
                                             start=(kc == 0), stop=(kc == 7))
                        return last
                    P.op("pe", mo, reads=[("ynT", t)] + [("wo", kc) for kc in range(8)], writes=[("ps", bank)])
                    P.op("dve", lambda v, half=half, bank=bank, xs=xs: v.tensor_tensor(
                        xs[:, half * 512:(half + 1) * 512], xs[:, half * 512:(half + 1) * 512], psf(bank), ALU.add),
                        reads=[("ps", bank), ("xh", xi)], writes=[("xh", xi)])
                col = 4 + t
                P.op("dve", lambda v, xs=xs, t=t, col=col: v.scalar_tensor_tensor(
                    out=hnb2[t], in0=xs, scalar=1.0, in1=xs, op0=ALU.mult, op1=ALU.mult,
                    accum_out=sm[:, col:col + 1]),
                    reads=[("xh", xi)], writes=[("ynbA", t, 0), ("ynbA", t, 1), ("sm", col)])
                rstd_ops(col, 1.0 / D)
                P.op("dve", lambda v, xs=xs, t=t, col=col: v.tensor_scalar(hnb2[t], xs, sm[:, col:col + 1], None, ALU.mult),
                     reads=[("xh", xi), ("sm", col)], writes=[("ynbA", t, 0), ("ynbA", t, 1)])

        def front_s3(G):
            for t in range(2):
                P.op("pe", transposes8(hnb2[t], None, None, TRB[t]), reads=[("ynbA", t, 0), ("ynbA", t, 1), "ident"], writes=[("ps", TRB[t])])
                P.op("act", lambda a, t=t: a.copy(hn2T[:, :, t * 128:(t + 1) * 128], psb(TRB[t]).rearrange("p (c t) -> p c t", c=8)),
                     reads=[("ps", TRB[t])], writes=[("hn2T", t)])

        def up(G):
            for fp in range(16):
                bank = 3 + fp % 2

                def mu(pe, fp=fp, bank=bank):
                    last = None
                    for j in range(2):
                        ffc = fp * 2 + j
                        for kc in range(8):
                            last = pe.matmul(psf(bank)[:, j * 256:(j + 1) * 256], wuT[:, kc, ffc * 128:(ffc + 1) * 128],
                                             hn2T[:, kc, :], start=(kc == 0), stop=(kc == 7))
                    return last
                P.op("pe", mu, reads=[("hn2T", 0), ("hn2T", 1), ("wu", fp // 2)], writes=[("ps", bank)])
                rs = fp % 2
                P.op("act", lambda a, rs=rs, bank=bank: a.activation(rtmp[rs], psf(bank), AF.Relu),
                     reads=[("ps", bank)], writes=[("stg", 2 + rs)])
                e = "dve"
                P.op(e, lambda v, rs=rs, fp=fp: v.tensor_tensor(
                    actT[:, fp * 2:fp * 2 + 2, :], rtmp[rs].rearrange("p (j t) -> p j t", j=2),
                    rtmp[rs].rearrange("p (j t) -> p j t", j=2), ALU.mult),
                    reads=[("stg", 2 + rs)], writes=[("actT", fp)])

        def down(G):
            for t in range(2):
                T = G * 2 + t
                xi = (G % 2) * 2 + t
                xs = xh[xi]
                for half in range(2):
                    bank = 5 + half

                    def md(pe, t=t, half=half, bank=bank):
                        last = None
                        for ffc in range(32):
                            last = pe.matmul(psf(bank), actT[:, ffc, t * 128:(t + 1) * 128],
                                             wdT[:, ffc, half * 512:(half + 1) * 512], start=(ffc == 0), stop=(ffc == 31))
                        return last
                    P.op("pe", md, reads=[("actT", i) for i in range(16)] + [("wd", i) for i in range(32)], writes=[("ps", bank)])
                    P.op("dve", lambda v, half=half, bank=bank, xs=xs: v.tensor_tensor(
                        xs[:, half * 512:(half + 1) * 512], xs[:, half * 512:(half + 1) * 512], psf(bank), ALU.add),
                        reads=[("ps", bank), ("xh", xi)], writes=[("xh", xi)])
                P.op("pool", lambda g, T=T, xs=xs: g.dma_start(out=h2scr[T * 128:(T + 1) * 128, :], in_=xs),
                     reads=[("xh", xi)], dma=("h2s", xi))


        front_s0(0)
        front_s1(0)
        front_s3(0)
        for G in range(ngroups):
            if G + 1 < ngroups:
                front_s0(G + 1)
            up(G)
            if G + 1 < ngroups:
                front_s1(G + 1)
            down(G)
            if G + 1 < ngroups:
                front_s3(G + 1)
    if stage >= 7 and opts.get('p4', True):
        P.barrier()
        L = Lay(ar, CONST_KEEP, ARENA)
        wgT = L.get([128, 8, D], BF16)
        wpT = L.get([128, 2, D], BF16)
        stg4 = [L.get([128, 2048], F32) for _ in range(2)]
        bgb = L.get([128, D], F32)
        gfb = L.get([128, D], F32)
        h2 = [L.get([128, D], F32) for _ in range(2)]
        pt_ = [L.get([128, PLE], F32) for _ in range(2)]
        hb = L.get([128, D], BF16)
        pb = L.get([128, PLE], BF16)
        h3T = L.get([128, 8, 128], BF16)
        pT = L.get([128, 2, 128], BF16)
        gt = L.get([128, D], F32)
        junk4 = L.get([128, D], F32)
        sm4 = L.get([128, 4], F32)
        ot = [L.get([128, D], F32) for _ in range(2)]
        cv = {"i": 0}
        for kc in range(8):
            s = kc % 2
            P.op("sp", lambda q, s=s, kc=kc: q.dma_start(out=stg4[s][:, 0:D], in_=w_gate[kc * 128:(kc + 1) * 128, :]),
                 writes=[("stg4", s)], dma=("stg4", s))
            P.op("dve", lambda v, s=s, kc=kc: v.tensor_scalar(wgT[:, kc, :], stg4[s][:, 0:D], gP[:, kc:kc + 1], None, ALU.mult),
                 reads=[("stg4", s), "gP"], writes=[("wg", kc)])
        for kc in range(2):
            s = kc % 2
            P.op("sp", lambda q, s=s, kc=kc: q.dma_start(out=stg4[s][:, 0:D], in_=w_proj[kc * 128:(kc + 1) * 128, :]),
                 writes=[("stg4", s)], dma=("stg4", s))
            P.op("dve", lambda v, s=s, kc=kc: v.tensor_copy(wpT[:, kc, :], stg4[s][:, 0:D]),
                 reads=[("stg4", s)], writes=[("wp", kc)])
        P.op("sp", lambda q: q.dma_start(out=bgb, in_=b_gate.partition_broadcast(128)[:, 0, :]), writes=["bgb"], dma="bgb")
        P.op("sp", lambda q: q.dma_start(out=gfb, in_=g_final.partition_broadcast(128)[:, 0, :]), writes=["gfb"], dma="gfb")

        def rstd4(col, scale):
            P.op("act", lambda a: a.activation(sm4[:, col:col + 1], sm4[:, col:col + 1], AF.Ln, bias=epsT, scale=scale),
                 reads=[("sm4", col), "eps"], writes=[("sm4", col)])
            P.op("act", lambda a: a.activation(sm4[:, col:col + 1], sm4[:, col:col + 1], AF.Exp, scale=-0.5),
                 reads=[("sm4", col)], writes=[("sm4", col)])

        ntiles = opts.get('ntiles', NT if stage >= 8 else 2)
        for T in range(ntiles):
            s = T % 2
            hs = h2[s]
            P.op("sp", lambda q, T=T, hs=hs: q.dma_start(out=hs, in_=h2scr[T * 128:(T + 1) * 128, :]),
                 writes=[("h2", s)], dma=("h2", s))
            P.op("sp", lambda q, T=T, s=s: q.dma_start(out=pt_[s], in_=pin[T * 128:(T + 1) * 128, :]),
                 writes=[("pt", s)], dma=("pt", s))
            P.op("dve", lambda v, hs=hs: v.scalar_tensor_tensor(
                out=junk4, in0=hs, scalar=1.0, in1=hs, op0=ALU.mult, op1=ALU.mult, accum_out=sm4[:, 0:1]),
                reads=[("h2", s)], writes=["junk4", ("sm4", 0)])
            rstd4(0, 1.0 / D)
            P.op("act", lambda a, hs=hs: a.activation(hb, hs, AF.Copy, scale=sm4[:, 0:1]),
                 reads=[("h2", s), ("sm4", 0)], writes=["hb"])
            P.op("act", lambda a, s=s: a.copy(pb, pt_[s]), reads=[("pt", s)], writes=["pb"])

            def tr4(pe):
                last = None
                for c in range(8):
                    last = pe.transpose(psb(0)[:, c * 128:(c + 1) * 128], hb[:, c * 128:(c + 1) * 128], ident)
                return last
            P.op("pe", tr4, reads=["hb", "ident"], writes=[("ps", 0)])
            P.op("act", lambda a: a.copy(h3T, psb(0).rearrange("p (c t) -> p c t", c=8)), reads=[("ps", 0)], writes=["h3T"])

            def tr5(pe):
                last = None
                for c in range(2):
                    last = pe.transpose(psb(1)[:, c * 128:(c + 1) * 128], pb[:, c * 128:(c + 1) * 128], ident)
                return last
            P.op("pe", tr5, reads=["pb", "ident"], writes=[("ps", 1)])
            P.op("act", lambda a: a.copy(pT, psb(1)[:, 0:256].rearrange("p (c t) -> p c t", c=2)), reads=[("ps", 1)], writes=["pT"])
            for half in range(2):
                gb_ = 2 + half
                pb_ = 4 + half
                cs = slice(half * 512, (half + 1) * 512)

                def mg(pe, gb_=gb_, cs=cs):
                    last = None
                    for kc in range(8):
                        last = pe.matmul(psf(gb_), h3T[:, kc, :], wgT[:, kc, cs], start=(kc == 0), stop=(kc == 7))
                    return last
                P.op("pe", mg, reads=["h3T"] + [("wg", kc) for kc in range(8)], writes=[("ps", gb_)])

                def mp(pe, pb_=pb_, cs=cs):
                    last = None
                    for kc in range(2):
                        last = pe.matmul(psf(pb_), pT[:, kc, :], wpT[:, kc, cs], start=(kc == 0), stop=(kc == 1))
                    return last
                P.op("pe", mp, reads=["pT", ("wp", 0), ("wp", 1)], writes=[("ps", pb_)])
                P.op("dve", lambda v, gb_=gb_, cs=cs: v.tensor_tensor(gt[:, cs], psf(gb_), bgb[:, cs], ALU.add),
                     reads=[("ps", gb_), "bgb"], writes=[("gt", half)])
                P.op("act", lambda a, cs=cs: a.activation(gt[:, cs], gt[:, cs], AF.Exp, scale=-1.0),
                     reads=[("gt", half)], writes=[("gt", half)])
                P.op("act", lambda a, cs=cs: a.activation(gt[:, cs], gt[:, cs], AF.Ln, bias=oneT, scale=1.0),
                     reads=[("gt", half), "one"], writes=[("gt", half)])
                P.op("act", lambda a, cs=cs: a.activation(gt[:, cs], gt[:, cs], AF.Exp, scale=-1.0),
                     reads=[("gt", half)], writes=[("gt", half)])
                P.op("dve", lambda v, cs=cs, pb_=pb_: v.tensor_tensor(gt[:, cs], gt[:, cs], psf(pb_), ALU.mult),
                     reads=[("gt", half), ("ps", pb_)], writes=[("gt", half)])
                P.op("dve", lambda v, cs=cs, hs=hs: v.tensor_tensor(hs[:, cs], hs[:, cs], gt[:, cs], ALU.add),
                     reads=[("gt", half), ("h2", s)], writes=[("h2", s)])
            P.op("dve", lambda v, hs=hs: v.scalar_tensor_tensor(
                out=junk4, in0=hs, scalar=1.0, in1=hs, op0=ALU.mult, op1=ALU.mult, accum_out=sm4[:, 1:2]),
                reads=[("h2", s)], writes=["junk4", ("sm4", 1)])
            rstd4(1, 1.0 / D)
            P.op("dve", lambda v, hs=hs, s=s: v.scalar_tensor_tensor(
                out=ot[s], in0=hs, scalar=sm4[:, 1:2], in1=gfb, op0=ALU.mult, op1=ALU.mult),
                reads=[("h2", s), ("sm4", 1), "gfb"], writes=[("ot", s)])
            P.op("pool", lambda g, T=T, s=s: g.dma_start(out=out[T * 128:(T + 1) * 128, :], in_=ot[s]),
                 reads=[("ot", s)], dma=("ot", s))

    dbg_out = {}
    P.barrier()
    for name, (ap, shape, dt) in dbg.items():
        dd = nc.dram_tensor("dbg_" + name, shape, dt, kind="ExternalOutput").ap()
        dbg_out[name] = dd
        P.op("sp", lambda q, dd=dd, ap=ap: q.dma_start(out=dd, in_=ap), dma=("dbg", name))
    P.barrier()
    P.finalize()

    import contextlib
    with contextlib.ExitStack() as es:
        esems = {e: es.enter_context(nc.semaphore("e_" + e)) for e in ("pe", "act", "dve", "pool")}
        esems["sp"] = None
        dsems = {}
        for i, k in enumerate(P.dma_count.keys()):
            dsems[k] = es.enter_context(nc.semaphore("d%d" % i))
        block = es.enter_context(nc.Block())

        @block.tensor
        def _(pe):
            P.emit("pe", pe, esems, dsems)

        @block.scalar
        def _(act):
            P.emit("act", act, esems, dsems)

        @block.vector
        def _(dve):
            P.emit("dve", dve, esems, dsems)

        @block.gpsimd
        def _(pool):
            P.emit("pool", pool, esems, dsems)

        @block.sync
        def _(sp):
            P.emit("sp", sp, esems, dsems)
    return nc, dbg_out


def make_in_maps(inputs):
    f = lambda a: np.ascontiguousarray(np.asarray(a, dtype=np.float32))
    x = f(inputs["x"])
    p = f(inputs["p"])[0]
    shared = {
        "g_attn": f(inputs["g_attn"]).reshape(1, D),
        "w_in": f(inputs["w_in"])[0],
        "g_out": np.concatenate([f(inputs["g_out_a"]).reshape(-1), f(inputs["g_out_b"]).reshape(-1)]).reshape(1, D),
        "w_out": f(inputs["w_out"])[0],
        "g_mlp": f(inputs["g_mlp"]).reshape(1, D),
        "w_up": f(inputs["w_up"])[0],
        "w_down": f(inputs["w_down"])[0],
        "g_ple": f(inputs["g_ple"]).reshape(1, D),
        "w_gate": f(inputs["w_ple_gate"])[0],
        "b_gate": f(inputs["b_ple_gate"]).reshape(1, D),
        "w_proj": f(inputs["w_ple_proj"])[0],
        "g_final": f(inputs["g_final"]).reshape(1, D),
    }
    maps = []
    for c in range(8):
        m = dict(shared)
        m["x"] = np.ascontiguousarray(x[c])
        m["p"] = np.ascontiguousarray(p[c])
        maps.append(m)
    return maps


def kernel(**inputs):
    nc, _ = build_nc()
    in_maps = make_in_maps(inputs)
    res = run_bass_kernel_spmd(nc, in_maps, core_ids=list(range(8)))
    return np.stack([np.asarray(r["out"], dtype=np.float32) for r in res.results], axis=0)
```

```python
import numpy as np
import concourse.bass as bass
import concourse.mybir as mybir
from concourse.bass_utils import run_bass_kernel_spmd

F32 = mybir.dt.float32
BF16 = mybir.dt.bfloat16
U8 = mybir.dt.uint8
ALU = mybir.AluOpType
AF = mybir.ActivationFunctionType
AX = mybir.AxisListType

S = 4096
D = 1024
NT = S // 128
DFF = 4096
PLE = 256
EPS = 1e-6
SLOPE_A = [2.0 ** (-(h + 0.5)) for h in range(8)]
SLOPE_B = [2.0 ** (-(h + 1.0)) for h in range(8)]
DILS = (1, 4, 16)
ENGS = ("pe", "act", "dve", "pool", "sp")


class Op:
    __slots__ = ("eng", "fn", "deps", "signal", "is_dma", "key", "val", "idx")


class Prog:
    def __init__(self):
        self.ops = {e: [] for e in ENGS}
        self.lastw = {}
        self.readers = {}
        self.dma_last = {}
        self.dma_count = {}

    def op(self, eng, fn, reads=(), writes=(), after=(), dma=None):
        o = Op()
        o.eng = eng
        o.fn = fn
        o.signal = False
        o.is_dma = dma is not None
        o.key = dma
        o.val = 0
        ps_reads = [b for b in reads if isinstance(b, tuple) and b[0] == "ps"]
        if ps_reads:
            reads = [b for b in reads if b not in ps_reads]
            writes = list(writes) + ps_reads
        cand = []
        for b in reads:
            w = self.lastw.get(b)
            if w is not None:
                cand.append(w)
        for b in writes:
            w = self.lastw.get(b)
            if w is not None:
                cand.append(w)
            cand.extend(self.readers.get(b, ()))
        cand.extend(after)
        if dma is not None:
            p = self.dma_last.get(dma)
            if p is not None:
                cand.append(p)
            self.dma_last[dma] = o
            self.dma_count[dma] = self.dma_count.get(dma, 0) + 1
            o.val = 16 * self.dma_count[dma]
            o.signal = True
        best = {}
        for d in cand:
            if d is o:
                continue
            if d.is_dma:
                k = ("dma", d.key)
                if k not in best or best[k].val < d.val:
                    best[k] = d
            else:
                if d.eng == "pe" and eng == "pe" and dma is None:
                    continue
                k = ("eng", d.eng)
                if k not in best or best[k].idx < d.idx:
                    best[k] = d
        o.deps = list(best.values())
        for d in o.deps:
            d.signal = True
        for b in reads:
            self.readers.setdefault(b, []).append(o)
        for b in writes:
            self.lastw[b] = o
            self.readers[b] = []
        o.idx = len(self.ops[eng])
        self.ops[eng].append(o)
        return o

    def barrier(self):
        lasts = []
        for e in ENGS:
            for o in reversed(self.ops[e]):
                if o.fn is not None and not o.is_dma:
                    lasts.append(o)
                    break
        lasts.extend(self.dma_last.values())
        for e in ENGS:
            self.op(e, None, after=lasts)
        self.lastw = {}
        self.readers = {}

    def finalize(self):
        for e in ENGS:
            n = 0
            for o in self.ops[e]:
                if o.is_dma:
                    continue
                if o.signal and o.fn is not None:
                    n += 1
                    o.val = n

    def emit(self, e, eng, esems, dsems):
        known = {}
        for o in self.ops[e]:
            for d in o.deps:
                if d.is_dma:
                    k = ("dma", d.key)
                    sem = dsems[d.key]
                else:
                    k = ("eng", d.eng)
                    sem = esems[d.eng]
                if known.get(k, 0) >= d.val:
                    continue
                eng.wait_ge(sem, d.val)
                known[k] = d.val
            if o.fn is None:
                continue
            last = o.fn(eng)
            if o.signal:
                if o.is_dma:
                    last.then_inc(dsems[o.key], 16)
                else:
                    last.then_inc(esems[e], 1)


class Arena:
    def __init__(self, nc, nbytes):
        self.t = nc.alloc_sbuf_tensor("arena", [128, nbytes], U8)
        self.n = nbytes

    def view(self, off, shape, dt):
        sz = 4 if dt == F32 else 2
        n = int(np.prod(shape[1:]))
        assert off % 4 == 0 and off + n * sz <= self.n, (off, shape, self.n)
        v = self.t[:, off:off + n * sz].bitcast(dt)
        if len(shape) > 2:
            names = " ".join("d%d" % i for i in range(1, len(shape)))
            kw = {"d%d" % i: shape[i] for i in range(1, len(shape))}
            v = v.rearrange("p (%s) -> p %s" % (names, names), **kw)
        return v


class Lay:
    def __init__(self, arena, start, limit):
        self.a = arena
        self.o = start
        self.limit = limit

    def get(self, shape, dt):
        sz = 4 if dt == F32 else 2
        n = int(np.prod(shape[1:])) * sz
        n = (n + 31) // 32 * 32
        v = self.a.view(self.o, shape, dt)
        self.o += n
        assert self.o <= self.limit, (self.o, self.limit)
        return v


def build_nc(stage=99, opts=None):
    opts = opts or {}
    nc = bass.Bass("TRN2", target_bir_lowering=False)
    P = Prog()

    def dram_in(name, shape):
        return nc.dram_tensor(name, shape, F32, kind="ExternalInput").ap()

    x = dram_in("x", [S, D])
    pin = dram_in("p", [S, PLE])
    g_attn = dram_in("g_attn", [1, D])
    w_in = dram_in("w_in", [D, 3 * D])
    g_out = dram_in("g_out", [1, D])
    w_out = dram_in("w_out", [D, D])
    g_mlp = dram_in("g_mlp", [1, D])
    w_up = dram_in("w_up", [D, DFF])
    w_down = dram_in("w_down", [DFF, D])
    g_ple = dram_in("g_ple", [1, D])
    w_gate = dram_in("w_gate", [D, D])
    b_gate = dram_in("b_gate", [1, D])
    w_proj = dram_in("w_proj", [PLE, D])
    g_final = dram_in("g_final", [1, D])
    out = nc.dram_tensor("out", [S, D], F32, kind="ExternalOutput").ap()
    skind = "ExternalOutput" if stage < 99 else "Internal"
    uscr = nc.dram_tensor("uscr", [4, S, 520], F32, kind=skind).ap()
    h2scr = nc.dram_tensor("h2scr", [S, D], F32, kind=skind).ap()

    ARENA = 212000
    ar = Arena(nc, ARENA)
    psall = nc.alloc_psum_tensor("psall", [128, 4096], F32)

    def psf(i):
        return psall[:, i * 512:(i + 1) * 512]

    def psb(i):
        return psall[:, i * 512:(i + 1) * 512].bitcast(BF16)

    L = Lay(ar, 0, 12288)
    ident = L.get([128, 128], BF16)
    epsT = L.get([128, 1], F32)
    oneT = L.get([128, 1], F32)
    gA = L.get([128, 8], F32)
    gM = L.get([128, 8], F32)
    gP = L.get([128, 8], F32)
    gO = L.get([128, 8], F32)
    CONST_KEEP = L.o
    D0 = L.get([128, 128], F32)
    Dpos = L.get([128, 128], F32)
    Dneg = L.get([128, 128], F32)
    D128 = L.get([128, 128], F32)
    Mcur = L.get([128, 128], F32)
    Mprev = L.get([128, 128], F32)
    facv = L.get([128, 2], F32)
    fac = L.get([128, 8, 2], F32)
    vgm = L.get([128, 32, 16], F32)
    negm = L.get([128, 32, 16], F32)
    CONST_END = L.o

    def cop(eng, fn, reads=(), writes=()):
        return P.op(eng, fn, reads, writes)

    cop("pool", lambda g: g.iota(D0, [[1, 128]], base=0, channel_multiplier=-1,
                                 allow_small_or_imprecise_dtypes=True), writes=["D0"])
    cop("pool", lambda g: g.iota(facv, [[128, 2]], base=-255, channel_multiplier=1,
                                 allow_small_or_imprecise_dtypes=True), writes=["facv"])
    cop("pool", lambda g: g.iota(vgm, [[128, 32], [-256, 16]], base=-255, channel_multiplier=1,
                                 allow_small_or_imprecise_dtypes=True), writes=["vgm0"])
    cop("pool", lambda g: g.iota(negm, [[1, 16], [0, 2], [-1, 16]], base=0, channel_multiplier=0,
                                 allow_small_or_imprecise_dtypes=True), writes=["negm0"])
    cop("dve", lambda v: v.memset(epsT, EPS), writes=["eps"])
    cop("dve", lambda v: v.memset(oneT, 1.0), writes=["one"])
    cop("dve", lambda v: v.tensor_scalar(ident, D0, 0.0, None, ALU.is_equal), reads=["D0"], writes=["ident"])
    cop("dve", lambda v: v.tensor_scalar(Dpos, D0, 0.0, None, ALU.max), reads=["D0"], writes=["Dpos"])
    cop("dve", lambda v: v.tensor_scalar(Dneg, D0, 0.0, 128.0, ALU.min, ALU.add), reads=["D0"], writes=["Dneg"])
    cop("dve", lambda v: v.tensor_scalar(D128, D0, 128.0, None, ALU.add), reads=["D0"], writes=["D128"])
    cop("dve", lambda v: v.tensor_scalar(Mcur, D0, 0.0, None, ALU.is_ge), reads=["D0"], writes=["Mcur"])
    cop("dve", lambda v: v.tensor_scalar(Mprev, D0, 0.0, None, ALU.is_le), reads=["D0"], writes=["Mprev"])
    cop("dve", lambda v: v.tensor_scalar(vgm, vgm, 0.0, None, ALU.max), reads=["vgm0"], writes=["vgm"])
    cop("dve", lambda v: v.tensor_scalar(negm, negm, 0.5, -1e30, ALU.is_lt, ALU.mult), reads=["negm0"], writes=["negm"])
    for h in range(8):
        cop("act", lambda a, h=h: a.activation(fac[:, h, :], facv, AF.Exp, scale=SLOPE_B[h]),
            reads=["facv"], writes=[("fac", h)])

    def gload(dst, src, key):
        P.op("sp", lambda q: q.dma_start(out=dst, in_=src.rearrange("o (c p) -> p (o c)", p=128),
                                         allow_slow_non_contiguous=True),
             writes=[key], dma=("g", key))

    gload(gA, g_attn, "gA")
    gload(gM, g_mlp, "gM")
    gload(gP, g_ple, "gP")
    gload(gO, g_out, "gO")

    dbg = {}

    L = Lay(ar, CONST_END, ARENA)
    hnT = L.get([128, 8, S], BF16)
    A0 = L.o
    xt = [L.get([128, D], F32) for _ in range(2)]
    hnb = [L.get([128, D], BF16) for _ in range(2)]
    ssq1 = L.get([128, NT], F32)
    rs1 = L.get([128, NT], F32)
    junk = L.get([128, D], F32)

    for T in range(NT):
        sl = T % 2
        P.op("sp", lambda q, T=T, sl=sl: q.dma_start(out=xt[sl], in_=x[T * 128:(T + 1) * 128, :]),
             writes=[("xt", sl)], dma=("xt", sl))
        P.op("dve", lambda v, T=T, sl=sl: v.scalar_tensor_tensor(
            out=junk, in0=xt[sl], scalar=1.0, in1=xt[sl], op0=ALU.mult, op1=ALU.mult,
            accum_out=ssq1[:, T:T + 1]), reads=[("xt", sl)], writes=["junk", ("ssq1", T)])
        P.op("act", lambda a, T=T: a.activation(rs1[:, T:T + 1], ssq1[:, T:T + 1], AF.Ln, bias=epsT, scale=1.0 / D),
             reads=[("ssq1", T), "eps"], writes=[("rs1", T)])
        P.op("act", lambda a, T=T: a.activation(rs1[:, T:T + 1], rs1[:, T:T + 1], AF.Exp, scale=-0.5),
             reads=[("rs1", T)], writes=[("rs1", T)])
        P.op("dve", lambda v, T=T, sl=sl: v.tensor_scalar(hnb[sl], xt[sl], rs1[:, T:T + 1], None, ALU.mult),
             reads=[("xt", sl), ("rs1", T)], writes=[("hnb", sl)])
        bank = T % 2

        def tr(pe, sl=sl, bank=bank):
            last = None
            for c in range(8):
                last = pe.transpose(psb(bank)[:, c * 128:(c + 1) * 128], hnb[sl][:, c * 128:(c + 1) * 128], ident)
            return last
        P.op("pe", tr, reads=[("hnb", sl), "ident"], writes=[("ps", bank)])
        src = psb(bank).rearrange("p (c t) -> p c t", c=8)
        dst = hnT[:, :, T * 128:(T + 1) * 128]
        if T % 2 == 0:
            P.op("act", lambda a, src=src, dst=dst: a.copy(dst, src), reads=[("ps", bank)], writes=[("hnT", T // 4)])
        else:
            P.op("dve", lambda v, src=src, dst=dst: v.tensor_copy(dst, src), reads=[("ps", bank)], writes=[("hnT", T // 4)])

    if stage == 1:
        dbg["hnT"] = (hnT, [128, 8, S], BF16)

    L = Lay(ar, A0, ARENA)
    wst = [L.get([128, 8, 384], F32) for _ in range(2)]
    wpr = [L.get([128, 8, 384], BF16) for _ in range(2)]
    QT = L.get([128, S], BF16)
    KT = L.get([128, S], BF16)
    VT = L.get([128, S], BF16)
    Vaug = L.get([128, 3, 32, 2, 65], BF16)
    PT = [L.get([128, 512], BF16) for _ in range(3)]
    WA = L.get([128, 4, 3, 512], BF16)
    WD = L.get([128, 8, 384], BF16)
    Uev = [L.get([128, 130], F32) for _ in range(4)]
    etmp = [L.get([128, 128], F32) for _ in range(2)]
    ksum = L.get([128, 16], F32)
    kmT = L.get([128, 16], BF16)
    gm = L.get([128, 2, 32, 16], F32)
    t8 = L.get([128, 2, 32, 8], F32)
    cT = L.get([128, 2, 32, 16], F32)
    accB = [L.get([128, 2, 2, 65], F32) for _ in range(2)]

    if stage >= 2:
        P.barrier()
        P.op("pool", lambda g: g.memset(Vaug[:, :, :, :, 64:65], 1.0), writes=["vones"])
        k = 0
        for pr in range(4):
            for bi, dil in enumerate(DILS):
                for hh in range(2):
                    sl_ = SLOPE_A[2 * pr + hh]
                    for which, (dsrc, msk, dkey, mkey) in enumerate(((Dpos, Mcur, "Dpos", "Mcur"), (Dneg, Mprev, "Dneg", "Mprev"))):
                        e = k % 2
                        k += 1
                        col = hh * 256 + which * 128
                        P.op("act", lambda a, e=e, dsrc=dsrc, sc=-sl_ * dil: a.activation(etmp[e], dsrc, AF.Exp, scale=sc),
                             reads=[dkey], writes=[("etmp", e)])
                        P.op("dve", lambda v, e=e, msk=msk, pr=pr, bi=bi, col=col: v.tensor_tensor(
                            WA[:, pr, bi, col:col + 128], etmp[e], msk, ALU.mult),
                            reads=[("etmp", e), mkey], writes=["WA"])
        for h in range(8):
            e = k % 2
            k += 1
            P.op("act", lambda a, e=e, h=h: a.activation(etmp[e], Dpos, AF.Exp, scale=-SLOPE_B[h]),
                 reads=["Dpos"], writes=[("etmp", e)])
            P.op("dve", lambda v, e=e, h=h: v.tensor_tensor(WD[:, h, 0:128], etmp[e], Mcur, ALU.mult),
                 reads=[("etmp", e), "Mcur"], writes=["WD"])
            P.op("dve", lambda v, e=e, h=h: v.tensor_tensor(WD[:, h, 256:384], etmp[e], Mcur, ALU.mult),
                 reads=[("etmp", e), "Mcur"], writes=["WD"])
            P.op("act", lambda a, h=h: a.activation(WD[:, h, 128:256], D128, AF.Exp, scale=-SLOPE_B[h]),
                 reads=["D128"], writes=["WD"])

    rot = {"proj": 0, "S": 0, "O": 0, "PT": 0, "U": 0, "ev": 0}

    def nxt(name, n):
        v = rot[name]
        rot[name] = (v + 1) % n
        return v

    def load_wpair(pi):
        slot = pi % 2
        grp, pr = divmod(pi, 4)
        base = grp * 1536 + pr * 128
        for w in range(3):
            c0 = base + w * 512
            P.op("sp", lambda q, slot=slot, w=w, c0=c0: q.dma_start(
                out=wst[slot][:, :, w * 128:(w + 1) * 128],
                in_=w_in[:, c0:c0 + 128].rearrange("(c p) n -> p c n", p=128)),
                writes=[("wst", slot, w)], dma=("wst", slot, w))
            eng = ("dve", "pool", "dve")[w]
            P.op(eng, lambda v, slot=slot, w=w: v.tensor_tensor(
                wpr[slot][:, :, w * 128:(w + 1) * 128], wst[slot][:, :, w * 128:(w + 1) * 128],
                gA.unsqueeze(2).to_broadcast([128, 8, 128]), ALU.mult),
                reads=[("wst", slot, w), "gA"], writes=[("wpr", slot, w)])

    def project(pi, is_b):
        slot = pi % 2
        for w, dst, key in ((0, QT, "QT"), (1, KT, "KT"), (2, VT, "VT")):
            for tc in range(8):
                bank = nxt("proj", 2)

                def mm(pe, slot=slot, w=w, tc=tc, bank=bank):
                    last = None
                    for kc in range(8):
                        last = pe.matmul(psf(bank), wpr[slot][:, kc, w * 128:(w + 1) * 128],
                                         hnT[:, kc, tc * 512:(tc + 1) * 512], start=(kc == 0), stop=(kc == 7))
                    return last
                P.op("pe", mm, reads=[("wpr", slot, w), ("hnT", tc)], writes=[("ps", bank)])
                d = dst[:, tc * 512:(tc + 1) * 512]
                if tc % 2 == 0:
                    P.op("act", lambda a, d=d, bank=bank: a.copy(d, psf(bank)), reads=[("ps", bank)], writes=[(key, tc)])
                else:
                    P.op("dve", lambda v, d=d, bank=bank: v.tensor_copy(d, psf(bank)), reads=[("ps", bank)], writes=[(key, tc)])
                if is_b and w == 1:
                    P.op("dve", lambda v, tc=tc, bank=bank: v.tensor_reduce(
                        ksum[:, tc * 2:(tc + 1) * 2], psf(bank).rearrange("p (a b) -> p a b", a=2), AX.X, ALU.add),
                        reads=[("ps", bank)], writes=[("ksum", tc)])

    def tok_ap(t, dil, r, n):
        st = r + 128 * dil * n
        return t[:, st:st + 127 * dil + 1:dil] if dil > 1 else t[:, st:st + 128]

    def blocks(dil):
        nb = 32 // dil
        return [(r, n) for r in range(dil) for n in range(nb)]

    def build_vaug(ords):
        for oi, dil in ords:
            bl = blocks(dil)
            for g0 in range(0, 32, 8):
                bank = nxt("ev", 2)

                def tr(pe, dil=dil, g0=g0, bank=bank, bl=bl):
                    last = None
                    for j in range(8):
                        r, n = bl[g0 + j]
                        last = pe.transpose(psb(bank)[:, j * 128:(j + 1) * 128], tok_ap(VT, dil, r, n), ident)
                    return last
                P.op("pe", tr, reads=[("VT", i) for i in range(8)] + ["ident"], writes=[("ps", bank)])
                src = psb(bank).rearrange("p (j h d) -> p j h d", j=8, h=2)
                dst = Vaug[:, oi, g0:g0 + 8, :, 0:64]
                if (g0 // 8) % 2 == 0:
                    P.op("dve", lambda v, src=src, dst=dst: v.tensor_copy(dst, src),
                         reads=[("ps", bank), "vones"], writes=[("Vaug", oi)])
                else:
                    P.op("act", lambda a, src=src, dst=dst: a.copy(dst, src),
                         reads=[("ps", bank), "vones"], writes=[("Vaug", oi)])

    allkeys = lambda name: [(name, i) for i in range(8)]

    def pipeline(iters, depth=2):
        n = len(iters)
        for i in range(n + depth):
            if i < n:
                iters[i][0]()
            if i - depth >= 0:
                iters[i - depth][1]()

    def attn_dilated(pr):
        iters = []
        for bi, dil in enumerate(DILS):
            bl = blocks(dil)
            bidx = {b: i for i, b in enumerate(bl)}
            for (r, n) in bl:
                ss = nxt("S", 2)
                sb0 = 2 + 2 * ss
                ob = 6 + nxt("O", 2)
                pt = nxt("PT", 3)
                us = nxt("U", 4)
                ncol = 256 if n > 0 else 128

                def stA(bi=bi, dil=dil, r=r, n=n, sb0=sb0, pt=pt, ncol=ncol):
                    def sc(pe):
                        last = None
                        for hh in range(2):
                            rows = slice(hh * 64, hh * 64 + 64)
                            qa = tok_ap(QT, dil, r, n)[rows, :]
                            last = pe.matmul(psf(sb0 + hh)[:, 0:128], tok_ap(KT, dil, r, n)[rows, :], qa,
                                             start=True, stop=True)
                            if n > 0:
                                last = pe.matmul(psf(sb0 + hh)[:, 128:256],
                                                 tok_ap(KT, dil, r, n - 1)[rows, :], qa, start=True, stop=True)
                        return last
                    P.op("pe", sc, reads=allkeys("QT") + allkeys("KT"), writes=[("ps", sb0), ("ps", sb0 + 1)])
                    src = psall[:, sb0 * 512:(sb0 + 2) * 512].rearrange("p (h c) -> p h c", h=2)[:, :, 0:ncol]
                    ptv = PT[pt].rearrange("p (h c) -> p h c", h=2)[:, :, 0:ncol]
                    wav = WA[:, pr, bi, :].rearrange("p (h c) -> p h c", h=2)[:, :, 0:ncol]
                    P.op("act", lambda a: a.activation(ptv, src, AF.Exp, scale=0.125),
                         reads=[("ps", sb0), ("ps", sb0 + 1)], writes=[("PT", pt)])
                    P.op("dve", lambda v: v.tensor_tensor(ptv, ptv, wav, ALU.mult),
                         reads=[("PT", pt), "WA"], writes=[("PT", pt)])

                def stB(bi=bi, dil=dil, r=r, n=n, ob=ob, pt=pt, us=us, bidx=bidx):
                    def pv(pe):
                        last = None
                        for hh in range(2):
                            o = psf(ob)[:, hh * 65:(hh + 1) * 65]
                            last = pe.matmul(o, PT[pt][:, hh * 256:hh * 256 + 128], Vaug[:, bi, bidx[(r, n)], hh, :],
                                             start=True, stop=(n == 0))
                            if n > 0:
                                last = pe.matmul(o, PT[pt][:, hh * 256 + 128:hh * 256 + 256],
                                                 Vaug[:, bi, bidx[(r, n - 1)], hh, :], start=False, stop=True)
                        return last
                    P.op("pe", pv, reads=[("PT", pt), ("Vaug", bi)], writes=[("ps", ob)])
                    P.op("dve", lambda v: v.tensor_copy(Uev[us], psf(ob)[:, 0:130]),
                         reads=[("ps", ob)], writes=[("Uev", us)])
                    st = r + 128 * dil * n
                    dst = uscr[bi, st:st + 127 * dil + 1:dil, pr * 130:(pr + 1) * 130] if dil > 1 else \
                        uscr[bi, st:st + 128, pr * 130:(pr + 1) * 130]
                    P.op("pool", lambda g: g.dma_start(out=dst, in_=Uev[us]),
                         reads=[("Uev", us)], dma=("Uev", us))
                iters.append((stA, stB))
        pipeline(iters)

    def attn_moba(pr):
        for hh in range(2):
            h = 2 * pr + hh
            for j in range(2):
                eng = "pool" if j == 0 else "dve"
                P.op(eng, lambda v, hh=hh, h=h, j=j: v.tensor_scalar(
                    Vaug[:, 1, j:32:2, hh, :], Vaug[:, 0, j:32:2, hh, :], fac[:, h, j:j + 1], None, ALU.mult),
                    reads=[("Vaug", 0), ("fac", h), "vones"], writes=[("Vaug", 1)])
        P.op("dve", lambda v: v.tensor_scalar(kmT, ksum, 1.0 / 256.0, None, ALU.mult),
             reads=[("ksum", i) for i in range(8)], writes=["kmT"])
        for hh in range(2):
            h = 2 * pr + hh
            rows = slice(hh * 64, hh * 64 + 64)
            gb = hh

            def gmm(pe, rows=rows, gb=gb):
                last = None
                for qt in range(32):
                    last = pe.matmul(psf(gb)[:, qt * 16:(qt + 1) * 16], QT[rows, qt * 128:(qt + 1) * 128], kmT[rows, :],
                                     start=True, stop=True)
                return last
            P.op("pe", gmm, reads=allkeys("QT") + ["kmT"], writes=[("ps", gb)])
            P.op("dve", lambda v, hh=hh, gb=gb: v.tensor_tensor(
                gm[:, hh], psf(gb).rearrange("p (a b) -> p a b", a=32), negm, ALU.add),
                reads=[("ps", gb), "negm"], writes=[("gm", hh)])

            def mx(v, hh=hh):
                last = None
                for qt in range(32):
                    last = v.max(t8[:, hh, qt, :], gm[:, hh, qt, :])
                return last
            P.op("dve", mx, reads=[("gm", hh)], writes=[("t8", hh)])
            P.op("act", lambda a, hh=hh, h=h: a.activation(cT[:, hh], vgm, AF.Exp, scale=-SLOPE_B[h]),
                 reads=["vgm"], writes=[("cT", hh)])
            P.op("dve", lambda v, hh=hh: v.tensor_tensor(
                gm[:, hh], gm[:, hh], t8[:, hh, :, 2:3].to_broadcast([128, 32, 16]), ALU.is_ge),
                reads=[("gm", hh), ("t8", hh)], writes=[("gm", hh)])
            P.op("dve", lambda v, hh=hh: v.tensor_tensor(cT[:, hh], cT[:, hh], gm[:, hh], ALU.mult),
                 reads=[("gm", hh), ("cT", hh)], writes=[("cT", hh)])
        iters = []
        nbq = opts.get('moba_nb', 16)
        for b in range(nbq):
            ab = b % 2
            for hh in range(2):
                h = 2 * pr + hh
                rows = slice(hh * 64, hh * 64 + 64)
                sb = 2 + nxt("S", 4)
                ob = 6 + nxt("O", 2)
                pt = nxt("PT", 3)

                def dA(b=b, rows=rows, sb=sb, pt=pt, h=h):
                    def scd(pe):
                        pe.matmul(psf(sb)[:, 0:256], KT[rows, b * 256:b * 256 + 128], QT[rows, b * 256:(b + 1) * 256],
                                  start=True, stop=True)
                        return pe.matmul(psf(sb)[:, 256:384], KT[rows, b * 256 + 128:(b + 1) * 256],
                                         QT[rows, b * 256 + 128:(b + 1) * 256], start=True, stop=True)
                    P.op("pe", scd, reads=allkeys("QT") + allkeys("KT"), writes=[("ps", sb)])
                    P.op("act", lambda a: a.activation(PT[pt][:, 0:384], psf(sb)[:, 0:384], AF.Exp, scale=0.125),
                         reads=[("ps", sb)], writes=[("PT", pt)])
                    P.op("dve", lambda v: v.tensor_tensor(PT[pt][:, 0:384], PT[pt][:, 0:384], WD[:, h, :], ALU.mult),
                         reads=[("PT", pt), "WD"], writes=[("PT", pt)])

                def dB(b=b, hh=hh, ob=ob, pt=pt, ab=ab):
                    def pvd(pe):
                        pe.matmul(psf(ob)[:, 0:65], PT[pt][:, 0:128], Vaug[:, 0, 2 * b, hh, :], start=True, stop=True)
                        pe.matmul(psf(ob)[:, 65:130], PT[pt][:, 128:256], Vaug[:, 0, 2 * b, hh, :], start=True, stop=False)
                        return pe.matmul(psf(ob)[:, 65:130], PT[pt][:, 256:384], Vaug[:, 0, 2 * b + 1, hh, :], start=False, stop=True)
                    P.op("pe", pvd, reads=[("PT", pt), ("Vaug", 0)], writes=[("ps", ob)])
                    P.op("dve", lambda v: v.tensor_copy(
                        accB[ab][:, :, hh, :], psf(ob)[:, 0:130].rearrange("p (j d) -> p j d", j=2)),
                        reads=[("ps", ob)], writes=[("acc", ab, hh)])
                iters.append((dA, dB))
                for n in range(b):
                    sb = 2 + nxt("S", 4)
                    ob = 6 + nxt("O", 2)
                    pt = nxt("PT", 3)

                    def oA(b=b, n=n, rows=rows, sb=sb, pt=pt):
                        def sco(pe):
                            q = QT[rows, b * 256:(b + 1) * 256]
                            pe.matmul(psf(sb)[:, 0:256], KT[rows, n * 256:n * 256 + 128], q, start=True, stop=True)
                            return pe.matmul(psf(sb)[:, 256:512], KT[rows, n * 256 + 128:(n + 1) * 256], q, start=True, stop=True)
                        P.op("pe", sco, reads=allkeys("QT") + allkeys("KT"), writes=[("ps", sb)])
                        P.op("act", lambda a: a.activation(PT[pt], psf(sb), AF.Exp, scale=0.125),
                             reads=[("ps", sb)], writes=[("PT", pt)])

                    def oB(b=b, n=n, hh=hh, ob=ob, pt=pt, ab=ab):
                        def pvo(pe):
                            last = None
                            for j in range(2):
                                o = psf(ob)[:, j * 65:(j + 1) * 65]
                                pe.matmul(o, PT[pt][:, j * 128:(j + 1) * 128], Vaug[:, 1, 2 * n, hh, :], start=True, stop=False)
                                last = pe.matmul(o, PT[pt][:, 256 + j * 128:256 + (j + 1) * 128], Vaug[:, 1, 2 * n + 1, hh, :],
                                                 start=False, stop=True)
                            return last
                        P.op("pe", pvo, reads=[("PT", pt), ("Vaug", 1)], writes=[("ps", ob)])

                        def accf(v):
                            last = None
                            for j in range(2):
                                last = v.scalar_tensor_tensor(
                                    out=accB[ab][:, j, hh, :], in0=psf(ob)[:, j * 65:(j + 1) * 65],
                                    scalar=cT[:, hh, 2 * b + j, n:n + 1], in1=accB[ab][:, j, hh, :],
                                    op0=ALU.mult, op1=ALU.add)
                            return last
                        P.op("dve", accf, reads=[("ps", ob), ("cT", hh), ("acc", ab, hh)], writes=[("acc", ab, hh)])
                    iters.append((oA, oB))

            def stA(): pass

            def stB(b=b, ab=ab):
                dst = uscr[3, b * 256:(b + 1) * 256, pr * 130:(pr + 1) * 130]
                P.op("pool", lambda g: g.dma_start(
                    out=dst.rearrange("(j p) c -> p j c", p=128), in_=accB[ab].rearrange("p j h d -> p j (h d)")),
                    reads=[("acc", ab, 0), ("acc", ab, 1)], dma=("acc", ab))
            iters.append((stA, stB))
        pipeline(iters)

    if stage >= 2:
        plist = opts.get('pairs', {2: [0], 3: [0, 1, 2, 3]}.get(stage, list(range(8))))
        load_wpair(plist[0])
        for ii, pi in enumerate(plist):
            if ii + 1 < len(plist):
                load_wpair(plist[ii + 1])
            is_b = pi >= 4
            project(pi, is_b)
            if stage == 2:
                dbg["QT"] = (QT, [128, S], BF16)
                dbg["KT"] = (KT, [128, S], BF16)
                dbg["VT"] = (VT, [128, S], BF16)
            if not is_b:
                build_vaug([(0, 1), (1, 4), (2, 16)])
                attn_dilated(pi)
            else:
                build_vaug([(0, 1)])
                attn_moba(pi - 4)

    if stage >= 5 and opts.get('p3', True):
        P.barrier()
        L = Lay(ar, CONST_KEEP, ARENA)
        woT = L.get([128, 8, D], BF16)
        wuT = L.get([128, 8, DFF], BF16)
        wdT = L.get([128, 32, D], BF16)
        stg = [L.get([128, 512], F32) for _ in range(2)]
        Ut = L.get([128, 4, 520], F32)
        xh = [L.get([128, D], F32) for _ in range(4)]
        ynb = L.get([128, D], BF16)
        ynT = L.get([128, 8, 128], BF16)
        hn2T = L.get([128, 8, 256], BF16)
        actT = L.get([128, 32, 256], BF16)
        rtmp = [L.get([128, 512], F32) for _ in range(2)]
        stg = stg + rtmp
        sm = L.get([128, 16], F32)
        rec = L.get([128, 16], F32)
        cv = {"i": 0}

        def conv_w(dst3, src2, nk, ncols, gt, key, colmajor=False):
            step = 512
            order = [(kc, c0) for c0 in range(0, ncols, step) for kc in range(nk)] if colmajor else \
                [(kc, c0) for kc in range(nk) for c0 in range(0, ncols, step)]
            for kc, c0 in order:
                w = min(step, ncols - c0)
                s = cv["i"] % 4
                e = ("dve", "act")[cv["i"] % 2]
                cv["i"] += 1
                wkey = (key, c0 // step) if colmajor else (key, kc)
                P.op("sp", lambda q, s=s, kc=kc, c0=c0, w=w: q.dma_start(
                    out=stg[s][:, 0:w], in_=src2[kc * 128:(kc + 1) * 128, c0:c0 + w]),
                    writes=[("stg", s)], dma=("stg", s))
                d = dst3[:, kc, c0:c0 + w]
                if gt is None:
                    if e == "act":
                        P.op(e, lambda a, s=s, d=d, w=w: a.copy(d, stg[s][:, 0:w]), reads=[("stg", s)], writes=[wkey])
                    else:
                        P.op(e, lambda v, s=s, d=d, w=w: v.tensor_copy(d, stg[s][:, 0:w]), reads=[("stg", s)], writes=[wkey])
                else:
                    gk, gkey = gt
                    sc = gk[:, kc:kc + 1]
                    if e == "act":
                        P.op(e, lambda a, s=s, d=d, w=w, sc=sc: a.activation(d, stg[s][:, 0:w], AF.Copy, scale=sc),
                             reads=[("stg", s), gkey], writes=[wkey])
                    else:
                        P.op(e, lambda v, s=s, d=d, w=w, sc=sc: v.tensor_scalar(d, stg[s][:, 0:w], sc, None, ALU.mult),
                             reads=[("stg", s), gkey], writes=[wkey])

        conv_w(woT, w_out, 8, D, (gO, "gO"), "wo")

        def rstd_ops(col, scale):
            P.op("act", lambda a: a.activation(sm[:, col:col + 1], sm[:, col:col + 1], AF.Ln, bias=epsT, scale=scale),
                 reads=[("sm", col), "eps"], writes=[("sm", col)])
            P.op("act", lambda a: a.activation(sm[:, col:col + 1], sm[:, col:col + 1], AF.Exp, scale=-0.5),
                 reads=[("sm", col)], writes=[("sm", col)])

        def transposes8(src_bf, dst3, dkey, bank):
            def tr(pe):
                last = None
                for c in range(8):
                    last = pe.transpose(psb(bank)[:, c * 128:(c + 1) * 128], src_bf[:, c * 128:(c + 1) * 128], ident)
                return last
            return tr

        ngroups = opts.get('ngroups', 16 if stage >= 6 else 1)
        ynbA = [ynb[:, 0:D], L.get([128, D], BF16)]
        ynTs = [ynT, L.get([128, 8, 128], BF16)]
        hnb2 = ynbA
        TRB = (0, 7)

        def front_s0(G):
            for t in range(2):
                T = G * 2 + t
                xi = (G % 2) * 2 + t
                xs = xh[xi]
                P.op("sp", lambda q, T=T: q.dma_start(
                    out=Ut, in_=uscr[:, T * 128:(T + 1) * 128, :].rearrange("s p c -> p s c")),
                    writes=["Ut"], dma="Ut")
                P.op("sp", lambda q, T=T, xs=xs: q.dma_start(out=xs, in_=x[T * 128:(T + 1) * 128, :]),
                     writes=[("xh", xi)], dma=("xh", xi))
                P.op("dve", lambda v: v.tensor_tensor(Ut[:, 0], Ut[:, 0], Ut[:, 1], ALU.add), reads=["Ut"], writes=["Ut"])
                P.op("dve", lambda v: v.tensor_tensor(Ut[:, 0], Ut[:, 0], Ut[:, 2], ALU.add), reads=["Ut"], writes=["Ut"])
                jk = Ut[:, 1, 0:512]
                for gi, us in ((0, 0), (1, 3)):
                    Uv = Ut[:, us].rearrange("p (h d) -> p h d", h=8)
                    Ov = Uv[:, :, 0:64]
                    col = t * 2 + gi
                    P.op("dve", lambda v, gi=gi, Uv=Uv: v.reciprocal(rec[:, gi * 8:(gi + 1) * 8], Uv[:, :, 64]),
                         reads=["Ut"], writes=[("rec", gi)])
                    P.op("dve", lambda v, gi=gi, Ov=Ov: v.tensor_tensor(
                        Ov, Ov, rec[:, gi * 8:(gi + 1) * 8].unsqueeze(2).to_broadcast([128, 8, 64]), ALU.mult),
                        reads=["Ut", ("rec", gi)], writes=["Ut"])
                    P.op("dve", lambda v, col=col, Ov=Ov, jk=jk: v.scalar_tensor_tensor(
                        out=jk.rearrange("p (h d) -> p h d", h=8), in0=Ov, scalar=1.0,
                        in1=Ov, op0=ALU.mult, op1=ALU.mult, accum_out=sm[:, col:col + 1]),
                        reads=["Ut"], writes=["Ut", ("sm", col)])
                    rstd_ops(col, 1.0 / 512)
                    P.op("dve", lambda v, gi=gi, col=col, Ov=Ov, t=t: v.tensor_scalar(
                        ynbA[t][:, gi * 512:(gi + 1) * 512].rearrange("p (h d) -> p h d", h=8), Ov, sm[:, col:col + 1], None, ALU.mult),
                        reads=["Ut", ("sm", col)], writes=[("ynbA", t, gi)])

        def front_s1(G):
            for t in range(2):
                P.op("pe", transposes8(ynbA[t], None, None, TRB[t]), reads=[("ynbA", t, 0), ("ynbA", t, 1), "ident"], writes=[("ps", TRB[t])])
                P.op("act", lambda a, t=t: a.copy(ynTs[t], psb(TRB[t]).rearrange("p (c t) -> p c t", c=8)),
                     reads=[("ps", TRB[t])], writes=[("ynT", t)])
            for t in range(2):
                xi = (G % 2) * 2 + t
                xs = xh[xi]
                for half in range(2):
                    bank = 1 + half

                    def mo(pe, half=half, bank=bank, t=t):
                        last = None
                        for kc in range(8):
                            last = pe.matmul(psf(bank), ynTs[t][:, kc, :], woT[:, kc, half * 512:(half + 1) * 512],
                                             start=(kc == 0), stop=(kc == 7))
                        return last
                    P.op("pe", mo, reads=[("ynT", t)] + [("wo", kc) for kc in range(8)], writes=[("ps", bank)])
                    P.op("dve", lambda v, half=half, bank=bank, xs=xs: v.tensor_tensor(
                        xs[:, half * 512:(half + 1) * 512], xs[:, half * 512:(half + 1) * 512], psf(bank), ALU.add),
                        reads=[("ps", bank), ("xh", xi)], writes=[("xh", xi)])
                col = 4 + t
                P.op("dve", lambda v, xs=xs, t=t, col=col: v.scalar_tensor_tensor(
                    out=hnb2[t], in0=xs, scalar=1.0, in1=xs, op0=ALU.mult, op1=ALU.mult,
                    accum_out=sm[:, col:col + 1]),
                    reads=[("xh", xi)], writes=[("ynbA", t, 0), ("ynbA", t, 1), ("sm", col)])
                rstd_ops(col, 1.0 / D)
                P.op("dve", lambda v, xs=xs, t=t, col=col: v.tensor_scalar(hnb2[t], xs, sm[:, col:col + 1], None, ALU.mult),
                     reads=[("xh", xi), ("sm", col)], writes=[("ynbA", t, 0), ("ynbA", t, 1)])

        def front_s3(G):
            for t in range(2):
                P.op("pe", transposes8(hnb2[t], None, None, TRB[t]), reads=[("ynbA", t, 0), ("ynbA", t, 1), "ident"], writes=[("ps", TRB[t])])
                P.op("act", lambda a, t=t: a.copy(hn2T[:, :, t * 128:(t + 1) * 128], psb(TRB[t]).rearrange("p (c t) -> p c t", c=8)),
                     reads=[("ps", TRB[t])], writes=[("hn2T", t)])

        def up(G):
            for fp in range(16):
                bank = 3 + fp % 2

                def mu(pe, fp=fp, bank=bank):
                    last = None
                    for j in range(2):
                        ffc = fp * 2 + j
                        for kc in range(8):
                            last = pe.matmul(psf(bank)[:, j * 256:(j + 1) * 256], wuT[:, kc, ffc * 128:(ffc + 1) * 128],
                                             hn2T[:, kc, :], start=(kc == 0), stop=(kc == 7))
                    return last
                P.op("pe", mu, reads=[("hn2T", 0), ("hn2T", 1), ("wu", fp // 2)], writes=[("ps", bank)])
                rs = fp % 2
                P.op("act", lambda a, rs=rs, bank=bank: a.activation(rtmp[rs], psf(bank), AF.Relu),
                     reads=[("ps", bank)], writes=[("stg", 2 + rs)])
                e = "dve"
                P.op(e, lambda v, rs=rs, fp=fp: v.tensor_tensor(
                    actT[:, fp * 2:fp * 2 + 2, :], rtmp[rs].rearrange("p (j t) -> p j t", j=2),
                    rtmp[rs].rearrange("p (j t) -> p j t", j=2), ALU.mult),
                    reads=[("stg", 2 + rs)], writes=[("actT", fp)])

        def down(G):
            for t in range(2):
                T = G * 2 + t
                xi = (G % 2) * 2 + t
                xs = xh[xi]
                for half in range(2):
                    bank = 5 + half

                    def md(pe, t=t, half=half, bank=bank):
                        last = None
                        for ffc in range(32):
                            last = pe.matmul(psf(bank), actT[:, ffc, t * 128:(t + 1) * 128],
                                             wdT[:, ffc, half * 512:(half + 1) * 512], start=(ffc == 0), stop=(ffc == 31))
                        return last
                    P.op("pe", md, reads=[("actT", i) for i in range(16)] + [("wd", i) for i in range(32)], writes=[("ps", bank)])
                    P.op("dve", lambda v, half=half, bank=bank, xs=xs: v.tensor_tensor(
                        xs[:, half * 512:(half + 1) * 512], xs[:, half * 512:(half + 1) * 512], psf(bank), ALU.add),
                        reads=[("ps", bank), ("xh", xi)], writes=[("xh", xi)])
                P.op("pool", lambda g, T=T, xs=xs: g.dma_start(out=h2scr[T * 128:(T + 1) * 128, :], in_=xs),
                     reads=[("xh", xi)], dma=("h2s", xi))


        front_s0(0)
        front_s1(0)
        front_s3(0)
        conv_w(wuT, w_up, 8, DFF, (gM, "gM"), "wu", colmajor=True)
        if ngroups > 1:
            front_s0(1)
        conv_w(wdT, w_down, 32, D, None, "wd")
        for G in range(ngroups):
            if G + 1 < ngroups and G >= 1:
                front_s0(G + 1)
            up(G)
            if G + 1 < ngroups:
                front_s1(G + 1)
            down(G)
            if G + 1 < ngroups:
                front_s3(G + 1)
    if stage >= 7 and opts.get('p4', True):
        P.barrier()
        L = Lay(ar, CONST_KEEP, ARENA)
        wgT = L.get([128, 8, D], BF16)
        wpT = L.get([128, 2, D], BF16)
        stg4 = [L.get([128, 2048], F32) for _ in range(2)]
        bgb = L.get([128, D], F32)
        gfb = L.get([128, D], F32)
        h2 = [L.get([128, D], F32) for _ in range(2)]
        pt_ = [L.get([128, PLE], F32) for _ in range(2)]
        hb = L.get([128, D], BF16)
        pb = L.get([128, PLE], BF16)
        h3T = L.get([128, 8, 128], BF16)
        pT = L.get([128, 2, 128], BF16)
        gt = L.get([128, D], F32)
        junk4 = L.get([128, D], F32)
        sm4 = L.get([128, 4], F32)
        ot = [L.get([128, D], F32) for _ in range(2)]
        cv = {"i": 0}
        for kc in range(8):
            s = kc % 2
            P.op("sp", lambda q, s=s, kc=kc: q.dma_start(out=stg4[s][:, 0:D], in_=w_gate[kc * 128:(kc + 1) * 128, :]),
                 writes=[("stg4", s)], dma=("stg4", s))
            P.op("dve", lambda v, s=s, kc=kc: v.tensor_scalar(wgT[:, kc, :], stg4[s][:, 0:D], gP[:, kc:kc + 1], None, ALU.mult),
                 reads=[("stg4", s), "gP"], writes=[("wg", kc)])
        for kc in range(2):
            s = kc % 2
            P.op("sp", lambda q, s=s, kc=kc: q.dma_start(out=stg4[s][:, 0:D], in_=w_proj[kc * 128:(kc + 1) * 128, :]),
                 writes=[("stg4", s)], dma=("stg4", s))
            P.op("dve", lambda v, s=s, kc=kc: v.tensor_copy(wpT[:, kc, :], stg4[s][:, 0:D]),
                 reads=[("stg4", s)], writes=[("wp", kc)])
        P.op("sp", lambda q: q.dma_start(out=bgb, in_=b_gate.partition_broadcast(128)[:, 0, :]), writes=["bgb"], dma="bgb")
        P.op("sp", lambda q: q.dma_start(out=gfb, in_=g_final.partition_broadcast(128)[:, 0, :]), writes=["gfb"], dma="gfb")

        def rstd4(col, scale):
            P.op("act", lambda a: a.activation(sm4[:, col:col + 1], sm4[:, col:col + 1], AF.Ln, bias=epsT, scale=scale),
                 reads=[("sm4", col), "eps"], writes=[("sm4", col)])
            P.op("act", lambda a: a.activation(sm4[:, col:col + 1], sm4[:, col:col + 1], AF.Exp, scale=-0.5),
                 reads=[("sm4", col)], writes=[("sm4", col)])

        ntiles = opts.get('ntiles', NT if stage >= 8 else 2)
        for T in range(ntiles):
            s = T % 2
            hs = h2[s]
            P.op("sp", lambda q, T=T, hs=hs: q.dma_start(out=hs, in_=h2scr[T * 128:(T + 1) * 128, :]),
                 writes=[("h2", s)], dma=("h2", s))
            P.op("sp", lambda q, T=T, s=s: q.dma_start(out=pt_[s], in_=pin[T * 128:(T + 1) * 128, :]),
                 writes=[("pt", s)], dma=("pt", s))
            P.op("dve", lambda v, hs=hs: v.scalar_tensor_tensor(
                out=junk4, in0=hs, scalar=1.0, in1=hs, op0=ALU.mult, op1=ALU.mult, accum_out=sm4[:, 0:1]),
                reads=[("h2", s)], writes=["junk4", ("sm4", 0)])
            rstd4(0, 1.0 / D)
            P.op("act", lambda a, hs=hs: a.activation(hb, hs, AF.Copy, scale=sm4[:, 0:1]),
                 reads=[("h2", s), ("sm4", 0)], writes=["hb"])
            P.op("act", lambda a, s=s: a.copy(pb, pt_[s]), reads=[("pt", s)], writes=["pb"])

            def tr4(pe):
                last = None
                for c in range(8):
                    last = pe.transpose(psb(0)[:, c * 128:(c + 1) * 128], hb[:, c * 128:(c + 1) * 128], ident)
                return last
            P.op("pe", tr4, reads=["hb", "ident"], writes=[("ps", 0)])
            P.op("act", lambda a: a.copy(h3T, psb(0).rearrange("p (c t) -> p c t", c=8)), reads=[("ps", 0)], writes=["h3T"])

            def tr5(pe):
                last = None
                for c in range(2):
                    last = pe.transpose(psb(1)[:, c * 128:(c + 1) * 128], pb[:, c * 128:(c + 1) * 128], ident)
                return last
            P.op("pe", tr5, reads=["pb", "ident"], writes=[("ps", 1)])
            P.op("act", lambda a: a.copy(pT, psb(1)[:, 0:256].rearrange("p (c t) -> p c t", c=2)), reads=[("ps", 1)], writes=["pT"])
            for half in range(2):
                gb_ = 2 + half
                pb_ = 4 + half
                cs = slice(half * 512, (half + 1) * 512)

                def mg(pe, gb_=gb_, cs=cs):
                    last = None
                    for kc in range(8):
                        last = pe.matmul(psf(gb_), h3T[:, kc, :], wgT[:, kc, cs], start=(kc == 0), stop=(kc == 7))
                    return last
                P.op("pe", mg, reads=["h3T"] + [("wg", kc) for kc in range(8)], writes=[("ps", gb_)])

                def mp(pe, pb_=pb_, cs=cs):
                    last = None
                    for kc in range(2):
                        last = pe.matmul(psf(pb_), pT[:, kc, :], wpT[:, kc, cs], start=(kc == 0), stop=(kc == 1))
                    return last
                P.op("pe", mp, reads=["pT", ("wp", 0), ("wp", 1)], writes=[("ps", pb_)])
                P.op("dve", lambda v, gb_=gb_, cs=cs: v.tensor_tensor(gt[:, cs], psf(gb_), bgb[:, cs], ALU.add),
                     reads=[("ps", gb_), "bgb"], writes=[("gt", half)])
                P.op("act", lambda a, cs=cs: a.activation(gt[:, cs], gt[:, cs], AF.Exp, scale=-1.0),
                     reads=[("gt", half)], writes=[("gt", half)])
                P.op("act", lambda a, cs=cs: a.activation(gt[:, cs], gt[:, cs], AF.Ln, bias=oneT, scale=1.0),
                     reads=[("gt", half), "one"], writes=[("gt", half)])
                P.op("act", lambda a, cs=cs: a.activation(gt[:, cs], gt[:, cs], AF.Exp, scale=-1.0),
                     reads=[("gt", half)], writes=[("gt", half)])
                P.op("dve", lambda v, cs=cs, pb_=pb_: v.tensor_tensor(gt[:, cs], gt[:, cs], psf(pb_), ALU.mult),
                     reads=[("gt", half), ("ps", pb_)], writes=[("gt", half)])
                P.op("dve", lambda v, cs=cs, hs=hs: v.tensor_tensor(hs[:, cs], hs[:, cs], gt[:, cs], ALU.add),
                     reads=[("gt", half), ("h2", s)], writes=[("h2", s)])
            P.op("dve", lambda v, hs=hs: v.scalar_tensor_tensor(
                out=junk4, in0=hs, scalar=1.0, in1=hs, op0=ALU.mult, op1=ALU.mult, accum_out=sm4[:, 1:2]),
                reads=[("h2", s)], writes=["junk4", ("sm4", 1)])
            rstd4(1, 1.0 / D)
            P.op("dve", lambda v, hs=hs, s=s: v.scalar_tensor_tensor(
                out=ot[s], in0=hs, scalar=sm4[:, 1:2], in1=gfb, op0=ALU.mult, op1=ALU.mult),
                reads=[("h2", s), ("sm4", 1), "gfb"], writes=[("ot", s)])
            P.op("pool", lambda g, T=T, s=s: g.dma_start(out=out[T * 128:(T + 1) * 128, :], in_=ot[s]),
                 reads=[("ot", s)], dma=("ot", s))

    dbg_out = {}
    P.barrier()
    for name, (ap, shape, dt) in dbg.items():
        dd = nc.dram_tensor("dbg_" + name, shape, dt, kind="ExternalOutput").ap()
        dbg_out[name] = dd
        P.op("sp", lambda q, dd=dd, ap=ap: q.dma_start(out=dd, in_=ap), dma=("dbg", name))
    P.barrier()
    P.finalize()

    import contextlib
    with contextlib.ExitStack() as es:
        esems = {e: es.enter_context(nc.semaphore("e_" + e)) for e in ("pe", "act", "dve", "pool")}
        esems["sp"] = None
        dsems = {}
        for i, k in enumerate(P.dma_count.keys()):
            dsems[k] = es.enter_context(nc.semaphore("d%d" % i))
        block = es.enter_context(nc.Block())

        @block.tensor
        def _(pe):
            P.emit("pe", pe, esems, dsems)

        @block.scalar
        def _(act):
            P.emit("act", act, esems, dsems)

        @block.vector
        def _(dve):
            P.emit("dve", dve, esems, dsems)

        @block.gpsimd
        def _(pool):
            P.emit("pool", pool, esems, dsems)

        @block.sync
        def _(sp):
            P.emit("sp", sp, esems, dsems)
    return nc, dbg_out


def make_in_maps(inputs):
    f = lambda a: np.ascontiguousarray(np.asarray(a, dtype=np.float32))
    x = f(inputs["x"])
    p = f(inputs["p"])[0]
    shared = {
        "g_attn": f(inputs["g_attn"]).reshape(1, D),
        "w_in": f(inputs["w_in"])[0],
        "g_out": np.concatenate([f(inputs["g_out_a"]).reshape(-1), f(inputs["g_out_b"]).reshape(-1)]).reshape(1, D),
        "w_out": f(inputs["w_out"])[0],
        "g_mlp": f(inputs["g_mlp"]).reshape(1, D),
        "w_up": f(inputs["w_up"])[0],
        "w_down": f(inputs["w_down"])[0],
        "g_ple": f(inputs["g_ple"]).reshape(1, D),
        "w_gate": f(inputs["w_ple_gate"])[0],
        "b_gate": f(inputs["b_ple_gate"]).reshape(1, D),
        "w_proj": f(inputs["w_ple_proj"])[0],
        "g_final": f(inputs["g_final"]).reshape(1, D),
    }
    maps = []
    for c in range(8):
        m = dict(shared)
        m["x"] = np.ascontiguousarray(x[c])
        m["p"] = np.ascontiguousarray(p[c])
        maps.append(m)
    return maps


def kernel(**inputs):
    nc, _ = build_nc()
    in_maps = make_in_maps(inputs)
    res = run_bass_kernel_spmd(nc, in_maps, core_ids=list(range(8)))
    return np.stack([np.asarray(r["out"], dtype=np.float32) for r in res.results], axis=0)
```
